# Optimizing a Trainium2 kernel written in Bass

```python
import jax, jax.numpy as jnp
from jax import lax
import numpy as np

D_MODEL = 1024
BATCH = 32
SEQ = 2048
DEPTH = 1

HEAD_DIM = 64
MOBA_HEADS = 8
MOBA_BLOCK = 256
MOBA_TOPK = 3
DSA_HEADS = 8
DSA_MAX_TOPK = 256
IDX_HEADS = 8
IDX_DIM = 64
N_GROUPS = 4
EXPERTS_PER_GROUP = 8
N_EXPERTS = N_GROUPS * EXPERTS_PER_GROUP
EXPERT_TOPK = 2
EXPERT_FF = 512
DISPATCH_BLOCK = 256
Q_CHUNK = 128
ROPE_THETA = 10000.0
RMS_EPS = 1e-6
NEG = -1e30

MOBA_WIDTH = MOBA_HEADS * HEAD_DIM
DSA_WIDTH = DSA_HEADS * HEAD_DIM
IDX_SCALE = (IDX_HEADS * IDX_DIM) ** -0.5
IN_SIZES = (MOBA_WIDTH,) * 3 + (DSA_WIDTH,) * 3 + (IDX_HEADS * IDX_DIM, IDX_DIM, IDX_HEADS, D_MODEL, D_MODEL)
IN_COLS = sum(IN_SIZES)
SPLIT_POINTS = tuple(int(v) for v in np.cumsum(IN_SIZES)[:-1])

kernel_name = "hybrid_moba_dsa_hier_moe_block"


def rms_norm(x, g):
    xf = x.astype(jnp.float32)
    y = xf * lax.rsqrt(jnp.mean(xf * xf, axis=-1, keepdims=True) + RMS_EPS)
    return (y * g.astype(jnp.float32)).astype(x.dtype)


def rope(x):
    s, d = x.shape[1], x.shape[-1]
    inv = jnp.power(ROPE_THETA, -jnp.arange(0, d, 2, dtype=jnp.float32) / d)
    ang = jnp.arange(s, dtype=jnp.float32)[:, None] * inv[None, :]
    ang = ang.reshape((s,) + (1,) * (x.ndim - 3) + (d // 2,))
    cos, sin = jnp.cos(ang), jnp.sin(ang)
    x1, x2 = jnp.split(x.astype(jnp.float32), 2, axis=-1)
    return jnp.concatenate([x1 * cos - x2 * sin, x1 * sin + x2 * cos], axis=-1).astype(x.dtype)


def masked_softmax(scores, mask):
    return jax.nn.softmax(jnp.where(mask, scores.astype(jnp.float32), NEG), axis=-1)


def moba_attend(q, k, v):
    h, s, d = q.shape
    nb = -(-s // MOBA_BLOCK)
    pad = nb * MOBA_BLOCK - s
    kb = jnp.pad(k, ((0, 0), (0, pad), (0, 0))).reshape(h, nb, MOBA_BLOCK, d)
    vb = jnp.pad(v, ((0, 0), (0, pad), (0, 0))).reshape(h, nb, MOBA_BLOCK, d)
    n_sel = max(1, min(MOBA_TOPK, nb - 1))
    own = jnp.arange(s) // MOBA_BLOCK
    k_mean = jnp.mean(kb.astype(jnp.float32), axis=2)
    gate = jnp.einsum('hsd,hnd->hsn', q.astype(jnp.float32), k_mean)
    past = jnp.arange(nb)[None, :] < own[:, None]
    gate = jnp.where(past[None], gate, NEG)
    _, sel = lax.top_k(gate, n_sel)
    sel_ok = sel < own[None, :, None]
    n_chunks = s // Q_CHUNK
    qc = q.reshape(h, n_chunks, Q_CHUNK, d).transpose(1, 0, 2, 3)
    selc = sel.reshape(h, n_chunks, Q_CHUNK, n_sel).transpose(1, 0, 2, 3)
    okc = sel_ok.reshape(h, n_chunks, Q_CHUNK, n_sel).transpose(1, 0, 2, 3)
    scale = d ** -0.5
    head_ix = jnp.arange(h)[:, None, None]

    def one_chunk(args):
        qi, si, oki, c = args
        q_pos = c * Q_CHUNK + jnp.arange(Q_CHUNK)
        blk = (c * Q_CHUNK) // MOBA_BLOCK
        k_sel = kb[head_ix, si]
        v_sel = vb[head_ix, si]
        k_own = lax.dynamic_index_in_dim(kb, blk, axis=1, keepdims=False)
        v_own = lax.dynamic_index_in_dim(vb, blk, axis=1, keepdims=False)
        s_sel = jnp.einsum('hqd,hqnkd->hqnk', qi, k_sel).reshape(h, Q_CHUNK, n_sel * MOBA_BLOCK) * scale
        s_own = jnp.einsum('hqd,hkd->hqk', qi, k_own) * scale
        m_sel = jnp.broadcast_to(oki[..., None], oki.shape + (MOBA_BLOCK,)).reshape(h, Q_CHUNK, n_sel * MOBA_BLOCK)
        k_pos = blk * MOBA_BLOCK + jnp.arange(MOBA_BLOCK)
        m_own = jnp.broadcast_to((k_pos[None, :] <= q_pos[:, None])[None], (h, Q_CHUNK, MOBA_BLOCK))
        p = masked_softmax(jnp.concatenate([s_sel, s_own], axis=-1),
                           jnp.concatenate([m_sel, m_own], axis=-1)).astype(v.dtype)
        p_sel = p[..., :n_sel * MOBA_BLOCK].reshape(h, Q_CHUNK, n_sel, MOBA_BLOCK)
        p_own = p[..., n_sel * MOBA_BLOCK:]
        return (jnp.einsum('hqnk,hqnkd->hqd', p_sel, v_sel)
                + jnp.einsum('hqk,hkd->hqd', p_own, v_own))

    out = lax.map(one_chunk, (qc, selc, okc, jnp.arange(n_chunks)))
    return out.transpose(1, 0, 2, 3).reshape(h, s, d)


def dsa_attend(q, k, v, q_idx, k_idx, w_idx):
    h, s, d = q.shape
    top = min(DSA_MAX_TOPK, s // 4)
    n_chunks = s // Q_CHUNK
    qc = q.reshape(h, n_chunks, Q_CHUNK, d).transpose(1, 0, 2, 3)
    qic = q_idx.reshape(n_chunks, Q_CHUNK, IDX_HEADS, IDX_DIM)
    wic = w_idx.reshape(n_chunks, Q_CHUNK, IDX_HEADS)
    k_pos = jnp.arange(s)
    kf = k_idx.astype(jnp.float32)
    scale = d ** -0.5

    def one_chunk(args):
        qi, qii, wi, c = args
        q_pos = c * Q_CHUNK + jnp.arange(Q_CHUNK)
        logits = jnp.einsum('qhd,sd->qhs', qii.astype(jnp.float32), kf)
        score = jnp.einsum('qh,qhs->qs', wi.astype(jnp.float32), jax.nn.relu(logits))
        score = jnp.where(k_pos[None, :] <= q_pos[:, None], score, NEG)
        _, idx = lax.top_k(score, top)
        ok = idx <= q_pos[:, None]
        k_sel = k[:, idx]
        v_sel = v[:, idx]
        sc = jnp.einsum('hqd,hqkd->hqk', qi, k_sel) * scale
        p = masked_softmax(sc, ok[None]).astype(v.dtype)
        return jnp.einsum('hqk,hqkd->hqd', p, v_sel)

    out = lax.map(one_chunk, (qc, qic, wic, jnp.arange(n_chunks)))
    return out.transpose(1, 0, 2, 3).reshape(h, s, d)


def hier_moe(h, w_group, b_group, w_expert, b_expert, w1, w3, w2):
    b, s, d = h.shape
    t = b * s
    xt = h.reshape(t, d)
    g_logit = (xt @ w_group + b_group).astype(jnp.float32)
    g_prob = jax.nn.softmax(g_logit, axis=-1)
    g_sel = jnp.argmax(g_logit, axis=-1)
    g_w = jnp.take_along_axis(g_prob, g_sel[:, None], axis=1)
    e_logit = (xt @ w_expert + b_expert).astype(jnp.float32).reshape(t, N_GROUPS, EXPERTS_PER_GROUP)
    e_logit = jnp.take_along_axis(e_logit, g_sel[:, None, None], axis=1)[:, 0]
    top_p, top_i = lax.top_k(jax.nn.softmax(e_logit, axis=-1), EXPERT_TOPK)
    gate = g_w * top_p / jnp.sum(top_p, axis=-1, keepdims=True)
    expert = g_sel[:, None] * EXPERTS_PER_GROUP + top_i
    n_asg = t * EXPERT_TOPK
    e_flat = expert.reshape(-1)
    order = jnp.argsort(e_flat)
    e_sorted = e_flat[order]
    tok_sorted = order // EXPERT_TOPK
    gate_sorted = gate.reshape(-1)[order].astype(h.dtype)
    counts = jnp.bincount(e_flat, length=N_EXPERTS)
    padded = (counts + DISPATCH_BLOCK - 1) // DISPATCH_BLOCK * DISPATCH_BLOCK
    ends = jnp.cumsum(padded)
    starts_pad = ends - padded
    starts = jnp.cumsum(counts) - counts
    dest = starts_pad[e_sorted] + jnp.arange(n_asg) - starts[e_sorted]
    n_blocks = -(-n_asg // DISPATCH_BLOCK) + N_EXPERTS
    xs = jnp.zeros((n_blocks * DISPATCH_BLOCK, d), h.dtype).at[dest].set(xt[tok_sorted])
    block_expert = jnp.minimum(
        jnp.searchsorted(ends, jnp.arange(n_blocks) * DISPATCH_BLOCK, side='right'), N_EXPERTS - 1)

    def expert_block(args):
        xb, e = args
        hid = jax.nn.silu(xb @ w1[e]) * (xb @ w3[e])
        return hid @ w2[e]

    ys = lax.map(expert_block, (xs.reshape(n_blocks, DISPATCH_BLOCK, d), block_expert)).reshape(-1, d)
    out = jax.ops.segment_sum(ys[dest] * gate_sorted[:, None], tok_sorted, num_segments=t)
    return out.reshape(b, s, d)


def hybrid_layer(x, g_mix, w_in, w_proj_a, w_proj_b, w_out, g_ffn,
                 w_group, b_group, w_expert, b_expert, w1, w3, w2):
    b, s, _ = x.shape
    h = rms_norm(x, g_mix)
    qa, ka, va, qb, kb, vb, qi, ki, wi, ga, gb = jnp.split(h @ w_in, SPLIT_POINTS, axis=-1)

    def heads(t, n):
        return t.reshape(b, s, n, HEAD_DIM)

    qa = rope(heads(qa, MOBA_HEADS)).transpose(0, 2, 1, 3)
    ka = rope(heads(ka, MOBA_HEADS)).transpose(0, 2, 1, 3)
    va = heads(va, MOBA_HEADS).transpose(0, 2, 1, 3)
    qb = rope(heads(qb, DSA_HEADS)).transpose(0, 2, 1, 3)
    kb = rope(heads(kb, DSA_HEADS)).transpose(0, 2, 1, 3)
    vb = heads(vb, DSA_HEADS).transpose(0, 2, 1, 3)
    qi = rope(qi.reshape(b, s, IDX_HEADS, IDX_DIM))
    ki = rope(ki)
    wi = wi * IDX_SCALE
    o_a = lax.map(lambda a: moba_attend(*a), (qa, ka, va))
    o_b = lax.map(lambda a: dsa_attend(*a), (qb, kb, vb, qi, ki, wi))
    o_a = o_a.transpose(0, 2, 1, 3).reshape(b, s, MOBA_WIDTH)
    o_b = o_b.transpose(0, 2, 1, 3).reshape(b, s, DSA_WIDTH)
    mixed = jax.nn.sigmoid(ga) * (o_a @ w_proj_a) + jax.nn.sigmoid(gb) * (o_b @ w_proj_b)
    x = x + mixed @ w_out
    x = x + hier_moe(rms_norm(x, g_ffn), w_group, b_group, w_expert, b_expert, w1, w3, w2)
    return x


def setup_inputs(seed: int = 0) -> dict:
    key = jax.random.key(seed)
    ks = jax.random.split(key, 16)
    nrm = jax.random.normal
    f32 = jnp.float32
    return {
        "x": nrm(ks[0], (BATCH, SEQ, D_MODEL), f32),
        "g_mix": 1.0 + 0.02 * nrm(ks[1], (DEPTH, D_MODEL), f32),
        "w_in": nrm(ks[2], (DEPTH, D_MODEL, IN_COLS), f32) * D_MODEL ** -0.5,
        "w_proj_a": nrm(ks[3], (DEPTH, MOBA_WIDTH, D_MODEL), f32) * MOBA_WIDTH ** -0.5,
        "w_proj_b": nrm(ks[4], (DEPTH, DSA_WIDTH, D_MODEL), f32) * DSA_WIDTH ** -0.5,
        "w_out": nrm(ks[5], (DEPTH, D_MODEL, D_MODEL), f32) * D_MODEL ** -0.5,
        "g_ffn": 1.0 + 0.02 * nrm(ks[6], (DEPTH, D_MODEL), f32),
        "w_group": nrm(ks[7], (DEPTH, D_MODEL, N_GROUPS), f32) * D_MODEL ** -0.5,
        "b_group": 0.01 * nrm(ks[8], (DEPTH, N_GROUPS), f32),
        "w_expert": nrm(ks[9], (DEPTH, D_MODEL, N_EXPERTS), f32) * D_MODEL ** -0.5,
        "b_expert": 0.01 * nrm(ks[10], (DEPTH, N_EXPERTS), f32),
        "w1": nrm(ks[11], (DEPTH, N_EXPERTS, D_MODEL, EXPERT_FF), f32) * D_MODEL ** -0.5,
        "w3": nrm(ks[12], (DEPTH, N_EXPERTS, D_MODEL, EXPERT_FF), f32) * D_MODEL ** -0.5,
        "w2": nrm(ks[13], (DEPTH, N_EXPERTS, EXPERT_FF, D_MODEL), f32) * EXPERT_FF ** -0.5,
        "g_final": 1.0 + 0.02 * nrm(ks[14], (D_MODEL,), f32),
    }


def reference(x, g_mix, w_in, w_proj_a, w_proj_b, w_out, g_ffn,
              w_group, b_group, w_expert, b_expert, w1, w3, w2, g_final):
    for l in range(DEPTH):
        x = hybrid_layer(x, g_mix[l], w_in[l], w_proj_a[l], w_proj_b[l], w_out[l], g_ffn[l],
                         w_group[l], b_group[l], w_expert[l], b_expert[l], w1[l], w3[l], w2[l])
    return rms_norm(x, g_final)
```

```python
import os
from contextlib import ExitStack
import numpy as np
import concourse.bass as bass
import concourse.mybir as mybir
from concourse.bass_utils import run_bass_kernel_spmd

F32 = mybir.dt.float32
BF16 = mybir.dt.bfloat16
I32 = mybir.dt.int32
ALU = mybir.AluOpType
AF = mybir.ActivationFunctionType
AX = mybir.AxisListType

NCORE = 8
SEQ = 2048
D = 1024
SPC = 4
NT = SPC * SEQ
CH = 512
NCH_SEQ = SEQ // CH
NEXP = 32
CAP = 768
NEGB = -30000.0
NIT = 13
IDX_SCALE = float((8 * 64) ** -0.5)
EPS = 1e-6
BIGIDX = 1.0e6

ENGS = ("pe", "act", "dve", "pool", "sp")


class Sched:
    EP = 30000

    def __init__(self):
        self.q = {e: [] for e in ENGS}
        self.cnt = {e: 0 for e in ENGS}
        self.seen = {e: {} for e in ENGS}
        self.w = {}
        self.r = {}
        self.dma_cnt = {}
        self.dma_last = {}
        self.regs = {}

    def _deps(self, reads, writes):
        deps = {}

        def add(t):
            if t is not None and deps.get(t[0], 0) < t[1]:
                deps[t[0]] = t[1]
        for k in reads:
            add(self.w.get(k))
        for k in writes:
            add(self.w.get(k))
            for sk, v in self.r.get(k, {}).items():
                add((sk, v))
        return deps

    def _waits(self, eng, deps):
        for sk, v in deps.items():
            if eng == "pe" and sk == "pe":
                continue
            if self.seen[eng].get(sk, 0) >= v:
                continue
            self.seen[eng][sk] = v
            self.q[eng].append(("wait", sk, v))

    def _commit(self, tok, reads, writes):
        for k in reads:
            d = self.r.setdefault(k, {})
            if d.get(tok[0], 0) < tok[1]:
                d[tok[0]] = tok[1]
        for k in writes:
            self.w[k] = tok
            self.r[k] = {}

    def pe_drain(self):
        if self.cnt["pe"] > 0 and self.seen["pe"].get("pe", 0) < self.cnt["pe"]:
            self.seen["pe"]["pe"] = self.cnt["pe"]
            self.q["pe"].append(("wait", "pe", self.cnt["pe"]))

    def op(self, eng, fn, reads=(), writes=()):
        psr = [k for k in reads if isinstance(k, tuple) and k[0] in ("ps", "psb")]
        if psr:
            reads = [k for k in reads if k not in psr]
            writes = list(writes) + psr
        self._waits(eng, self._deps(reads, writes))
        self.cnt[eng] += 1
        tok = (eng, self.cnt[eng])
        self.q[eng].append(("op", fn, tok))
        self._commit(tok, reads, writes)

    def dma(self, eng, fn, key, reads=(), writes=()):
        deps = self._deps(reads, writes)
        sk = ("dma", key)
        if key in self.dma_last:
            t = self.dma_last[key]
            if deps.get(t[0], 0) < t[1]:
                deps[t[0]] = t[1]
        self._waits(eng, deps)
        self.dma_cnt[key] = self.dma_cnt.get(key, 0) + 16
        tok = (sk, self.dma_cnt[key])
        self.dma_last[key] = tok
        self.q[eng].append(("dma", fn, tok))
        self._commit(tok, reads, writes)

    def barrier(self):
        deps = {}
        for e in ("pe", "act", "dve", "pool"):
            if self.cnt[e] > 0:
                deps[e] = self.cnt[e]
        for key, c in self.dma_cnt.items():
            deps[("dma", key)] = c
        for e in ENGS:
            self._waits(e, dict(deps))

    def emit(self, nc, stack):
        if not hasattr(self, "sems"):
            self.sems = {}
        sems = self.sems

        def sem_of(sk, v):
            if isinstance(sk, tuple):
                if sk not in sems:
                    sems[sk] = stack.enter_context(nc.semaphore("d%d" % len(sems)))
                return sems[sk], v
            ep = (v - 1) // self.EP
            k2 = (sk, ep)
            if k2 not in sems:
                sems[k2] = stack.enter_context(nc.semaphore("c%d" % len(sems)))
            return sems[k2], (v - 1) % self.EP + 1

        for e in ENGS:
            for it in self.q[e]:
                if it[0] == "wait":
                    sem_of(it[1], it[2])
                else:
                    sem_of(it[2][0], it[2][1])
        print("[kernel] block instr counts", {e: len(self.q[e]) for e in ENGS}, "sems", len(sems), flush=True)
        with nc.Block() as block:
            def run(engname):
                def body(e):
                    if engname == "pool":
                        self.regs["bc"] = e.to_reg(NEXP * CAP - 1)
                    for ii, it in enumerate(self.q[engname]):
                      try:
                        if it[0] == "wait":
                            s, v = sem_of(it[1], it[2])
                            e.wait_ge(s, v)
                        elif it[0] == "op":
                            s, _ = sem_of(it[2][0], it[2][1])
                            it[1](e).then_inc(s, 1)
                        else:
                            s, _ = sem_of(it[2][0], it[2][1])
                            it[1](e).then_inc(s, 16)
                      except Exception:
                        print("[kernel] emit failure at", engname, ii, it[0], it[2] if it[0] != "wait" else it[1:], flush=True)
                        print([ (x[0], x[2] if x[0] != "wait" else x[1:]) for x in self.q[engname][max(0, ii - 6):ii]], flush=True)
                        raise
                return body
            block.tensor(run("pe"))
            block.scalar(run("act"))
            block.vector(run("dve"))
            block.gpsimd(run("pool"))
            block.sync(run("sp"))
        for e in ENGS:
            self.q[e] = []
        return len(sems)


def build(n_chunks=SPC * NCH_SEQ, do_moe=True, dbg=False, p3_tiles=None, skip2=False, lvl=99):
    nc = bass.Bass("TRN2", target_bir_lowering=False)
    S = Sched()

    def din(name, shape, dt=F32):
        return nc.dram_tensor(name, shape, dt, kind="ExternalInput").ap()

    x_d = din("x", [NT, D])
    wcat_d = din("wcat", [16, 128, 4096])
    nexp_in = NEXP if do_moe else 1
    w1_d = din("w1r", [nexp_in, 128, 4096])
    w3_d = din("w3r", [nexp_in, 128, 4096])
    w2_d = din("w2r", [nexp_in, 128, 4096])
    wr_d = din("wr", [128, 8 * 36])
    br_d = din("br", [128, 36])
    gv_d = din("gv", [3, 128, D])
    cos_d = din("cosr", [128, SEQ])
    sin_d = din("sinr", [128, SEQ])
    cst_d = din("cst", [128, 8, 128])
    ind_d = din("ind", [128, 8, 128])
    eoff_d = din("eoff", [128, 32])
    pow2_d = din("pow2", [128, NIT])
    out_d = nc.dram_tensor("out", [NT, D], F32, kind="ExternalOutput").ap()
    wsc_d = nc.dram_tensor("wsc", [16, 128, 4096], BF16, kind="Internal").ap()
    x1s_d = nc.dram_tensor("x1s", [NT, D], F32, kind=("ExternalOutput" if dbg else "Internal")).ap()
    xs_d = nc.dram_tensor("xs", [NEXP * CAP, D], BF16, kind="Internal").ap()
    ys_d = nc.dram_tensor("ys", [NEXP * CAP, D], F32, kind="Internal").ap()

    stack = ExitStack()
    with stack:
        def sb(name, shape, dt):
            return stack.enter_context(nc.sbuf_tensor(name, shape, dt))

        def pst(name, shape, dt):
            return stack.enter_context(nc.psum_tensor(name, shape, dt))

        P = [pst("ps%d" % i, [128, 512], F32) for i in range(7)]
        PB = pst("psb", [128, 1024], BF16)

        def PK(i):
            return ("ps", i)
        PBK = ("psb",)

        cstf = sb("cstf", [128, 8, 128], F32)
        cstb = sb("cstb", [128, 5, 128], BF16)
        indb = sb("indb", [128, 8, 128], BF16)
        eoff = sb("eoff_sb", [128, 32], F32)
        pow2 = sb("pow2_sb", [128, NIT], F32)
        wr = sb("wr_sb", [128, 8, 36], F32)
        brs = sb("br_sb", [128, 36], F32)
        epsb = sb("epsb", [128, 1], F32)
        GATES = sb("gates", [128, NT // 128, 2], F32)
        DEST = sb("dest", [128, NT // 128, 2], I32)
        basec = sb("basec", [128, 32], F32)
        ident_b = cstb[:, 0, :]
        perm_b = cstb[:, 1, :]
        tri_b = cstb[:, 2, :]
        su_b = cstb[:, 3, :]
        ones_b = cstb[:, 4, :]
        ident_f = cstf[:, 0, :]
        tri_tok = cstf[:, 5, :]

        S.dma("sp", lambda e: e.dma_start(out=cstf[:], in_=cst_d[:, :, :]), "cstf", writes=["cstf"])
        S.dma("sp", lambda e: e.dma_start(out=eoff[:], in_=eoff_d[:, :]), "eoff", writes=["eoff"])
        S.dma("sp", lambda e: e.dma_start(out=pow2[:], in_=pow2_d[:, :]), "pow2", writes=["pow2"])
        S.dma("sp", lambda e: e.dma_start(out=wr[:].rearrange("p a b -> p (a b)"), in_=wr_d[:, :]), "wr", writes=["wr"])
        S.dma("sp", lambda e: e.dma_start(out=brs[:], in_=br_d[:, :]), "brs", writes=["brs"])
        S.op("dve", lambda e: e.tensor_copy(out=cstb[:], in_=cstf[:, 0:5, :]), reads=["cstf"], writes=["cstb"])
        S.op("dve", lambda e: e.memset(epsb[:], EPS), writes=["epsb"])
        S.op("dve", lambda e: e.memset(basec[:], 0.0), writes=["basec"])

        ph1 = ExitStack()
        with ph1:
            def sb1(name, shape, dt):
                return ph1.enter_context(nc.sbuf_tensor(name, shape, dt))
            KA = sb1("KA", [128, 4, SEQ], BF16)
            KB = sb1("KB", [128, 4, SEQ], BF16)
            KI = sb1("KI", [128, SEQ], BF16)
            VA = sb1("VA", [128, 16, 12, 64], BF16)
            VB = sb1("VB", [128, 16, 12, 64], BF16)
            kmT = sb1("kmT", [128, 4, 8], BF16)
            kmf = sb1("kmf", [128, 4, 2], F32)
            hT = sb1("hT", [128, 8, CH], BF16)
            QAB = sb1("QAB", [128, 8, CH], BF16)
            QA = QAB[:, 0:4, :]
            QB = QAB[:, 4:8, :]
            mixT = QAB
            QI = sb1("QI", [128, 4, CH], BF16)
            rden = sb1("rden", [128, CH], F32)
            OA = sb1("OA", [128, 4, CH], BF16)
            OB = sb1("OB", [128, 4, CH], BF16)
            wbuf = [sb1("wbuf%d" % i, [128, 4096], BF16) for i in range(2)]
            XT = [sb1("xt%d" % i, [128, D], F32) for i in range(2)]
            hn = sb1("hn", [128, D], BF16)
            xnb = [sb1("xnb0", [128, D], BF16)] * 2
            gmix = sb1("gmix", [128, D], F32)
            gffn = sb1("gffn", [128, D], F32)
            cosc = sb1("cosc", [128, CH], F32)
            sinc = sb1("sinc", [128, CH], F32)
            xb = sb1("xb", [128, CH], BF16)
            PT = [sb1("pT%d" % i, [128, CH], BF16) for i in range(3)]
            RL = [sb1("RL%d" % i, [128, CH], BF16) for i in range(2)]
            Dw = sb1("Dw", [128, 8, 128], BF16)
            Ibuf = sb1("Ibuf", [128, SEQ], F32)
            maskb = sb1("maskb", [128, SEQ], BF16)
            wis = sb1("wis", [128, 4, 8], F32)
            st = sb1("st", [128, 16], F32)
            stp = sb1("stp", [128, NIT], F32)
            gsb = sb1("gsb", [128, 8, 8], F32)
            gtop = sb1("gtop", [128, 8, 8], F32)
            gtmp = sb1("gtmp", [128, 8, 8], F32)
            BT = sb1("BT", [128, 3, 4, 32], BF16)
            BIAS = sb1("BIAS", [128, 3, CH], BF16)
            rt = sb1("rt", [128, 256], F32)
            a12 = sb1("a12", [128, 32], BF16)
            U = sb1("U", [128, 4096], F32)
            Ub = U[:].bitcast(BF16)

            def UK(lo, hi):
                return [("U", i) for i in range(lo, hi)]

            def maskT(kt, c0, c1):
                return Ub[:, kt * 512 + c0: kt * 512 + c1]

            def fsc(i):
                return U[:, i * 512:(i + 1) * 512]
            xnf = U[:, 2048:3072]
            xnT = U[:, 3072:4096]

            S.dma("sp", lambda e: e.dma_start(out=gmix[:], in_=gv_d[0, :, :]), "gmix", writes=["gmix"])
            S.dma("sp", lambda e: e.dma_start(out=gffn[:], in_=gv_d[1, :, :]), "gffn", writes=["gffn"])
            S.dma("sp", lambda e: e.dma_start(out=U[:, 0:1024].rearrange("p (a b) -> p a b", a=8), in_=ind_d[:, :, :]),
                  "U0", writes=UK(0, 4))
            S.op("dve", lambda e: e.tensor_copy(out=indb[:], in_=U[:, 0:1024].rearrange("p (a b) -> p a b", a=8)),
                 reads=UK(0, 4), writes=["indb"])
            S.op("pool", lambda e: e.memset(VA[:, :, 1:12:3, :], 1.0), writes=["VA1"])
            S.op("pool", lambda e: e.memset(VB[:, :, 1:12:3, :], 1.0), writes=["VB1"])
            S.op("pool", lambda e: e.memset(BT[:], 0.0), writes=["BT"])
            for g in range(16):
                wb = wbuf[g % 2]
                S.dma("sp", lambda e, g=g: e.dma_start(out=U[:], in_=wcat_d[g, :, :]), "U0",
                      writes=UK(0, 16))
                eng = ("act", "dve", "pool")[g % 3]
                if eng == "act":
                    S.op("act", lambda e, wb=wb: e.activation(out=wb[:], in_=U[:], func=AF.Copy),
                         reads=UK(0, 16), writes=[("wb", g % 2)])
                else:
                    S.op(eng, lambda e, wb=wb: e.tensor_copy(out=wb[:], in_=U[:]),
                         reads=UK(0, 16), writes=[("wb", g % 2)])
                S.dma("sp", lambda e, g=g, wb=wb: e.dma_start(out=wsc_d[g, :, :], in_=wb[:]), ("wbs", g % 2),
                      reads=[("wb", g % 2)], writes=[("wsc", g)])

            S.op("pool", lambda e: e.memset(U[:], 0.0), reads=UK(0, 16), writes=UK(0, 16))
            for zi in range(NEXP * CAP // 1024):
                S.dma("sp", lambda e, zi=zi: e.dma_start(
                    out=xs_d[zi * 1024:(zi + 1) * 1024, :].rearrange("(a p) d -> p a d", p=128),
                    in_=Ub[:, :].rearrange("p (a d) -> p a d", a=8)), ("xsz", zi % 4), reads=UK(0, 16), writes=[("xs",)])
            wstate = {"i": 0}

            def load_w(g):
                k = wstate["i"] % 2
                wstate["i"] += 1
                wb = wbuf[k]
                S.dma("sp", lambda e: e.dma_start(out=wb[:], in_=wsc_d[g, :, :]), ("wbl", k),
                      reads=[("wsc", g)], writes=[("wb", k)])
                return wb, ("wb", k)

            def mm(out, lhsT, rhs, start, stop, reads, writes):
                S.op("pe", lambda e: e.matmul(out, lhsT=lhsT, rhs=rhs, start=start, stop=stop),
                     reads=reads, writes=writes)

            def tr(out, in_, ident, reads, writes):
                S.op("pe", lambda e: e.transpose(out, in_, ident), reads=reads, writes=writes)

            def act(out, in_, func, reads, writes, **kw):
                S.op("act", lambda e: e.activation(out=out, in_=in_, func=func, **kw), reads=reads, writes=writes)

            def tt(eng, out, in0, in1, op, reads, writes):
                S.op(eng, lambda e: e.tensor_tensor(out=out, in0=in0, in1=in1, op=op), reads=reads, writes=writes)

            def ts(eng, out, in0, s1, s2, op0, op1, reads, writes, **kw):
                if op1 is None:
                    S.op(eng, lambda e: e.tensor_scalar(out=out, in0=in0, scalar1=s1, scalar2=None, op0=op0, **kw),
                         reads=reads, writes=writes)
                else:
                    S.op(eng, lambda e: e.tensor_scalar(out=out, in0=in0, scalar1=s1, scalar2=s2, op0=op0, op1=op1, **kw),
                         reads=reads, writes=writes)

            def stt(out, in0, scalar, in1, op0, op1, reads, writes, **kw):
                S.op("dve", lambda e: e.scalar_tensor_tensor(out=out, in0=in0, scalar=scalar, in1=in1, op0=op0, op1=op1, **kw),
                     reads=reads, writes=writes)

            def rms_stats(src, srckey, col):
                act(hn[:], src, AF.Square, reads=[srckey], writes=["hn", ("st", col)],
                    accum_out=st[:, col:col + 1])
                act(st[:, col + 1:col + 2], st[:, col:col + 1], AF.Ln, reads=[("st", col), "epsb"],
                    writes=[("st", col + 1)], scale=1.0 / D, bias=epsb[:, 0:1])
                act(st[:, col:col + 1], st[:, col + 1:col + 2], AF.Exp, reads=[("st", col + 1)],
                    writes=[("st", col)], scale=-0.5)

            for ci in range(n_chunks):
                s_i = ci // NCH_SEQ
                c = ci % NCH_SEQ
                tok0 = ci * CH
                p0 = c * CH
                S.dma("sp", lambda e, p0=p0: e.dma_start(out=cosc[:], in_=cos_d[:, p0:p0 + CH]), "cosc", writes=["cosc"])
                S.dma("sp", lambda e, p0=p0: e.dma_start(out=sinc[:], in_=sin_d[:, p0:p0 + CH]), "sinc", writes=["sinc"])
                if lvl < 1:
                    continue
                for j in range(4):
                    xt = XT[j % 2]
                    xk = ("xt", j % 2)
                    r0 = tok0 + j * 128
                    S.dma("sp", lambda e, xt=xt, r0=r0: e.dma_start(out=xt[:], in_=x_d[r0:r0 + 128, :]), xk, writes=[xk])
                    rms_stats(xt[:], xk, 0)
                    stt(hn[:], xt[:], st[:, 0:1], gmix[:], ALU.mult, ALU.mult, reads=[xk, ("st", 0), "gmix"], writes=["hn"])
                    for kc in range(8):
                        tr(PB[:, kc * 128:(kc + 1) * 128], hn[:, kc * 128:(kc + 1) * 128], ident_b,
                           reads=["hn", "cstb"], writes=[PBK])
                    act(hT[:, :, j * 128:(j + 1) * 128], PB[:, :].rearrange("p (a b) -> p a b", a=8), AF.Copy,
                        reads=[PBK], writes=[("hT", j)])
                hTk = [("hT", j) for j in range(4)]

                accb = {"i": 0}

                def next_acc():
                    accb["i"] ^= 1
                    return accb["i"]

                def proj_fm(wb, wk, m):
                    b = next_acc()
                    for kc in range(8):
                        mm(P[b][:, :], wb[:, kc * 512 + m * 128: kc * 512 + (m + 1) * 128], hT[:, kc, :],
                           kc == 0, kc == 7, reads=[wk] + hTk, writes=[PK(b)])
                    return b

                def rope(b, out_ap, outkeys):
                    act(xb[:], P[b][:, :], AF.Copy, reads=[PK(b)], writes=["xb"])
                    mm(P[2][:, :], perm_b, xb[:], True, True, reads=["xb", "cstb"], writes=[PK(2)])
                    tt("dve", fsc(0), P[b][:, :], cosc[:], ALU.mult, reads=[PK(b), "cosc"], writes=UK(0, 2))
                    tt("dve", fsc(1), P[2][:, :], sinc[:], ALU.mult, reads=[PK(2), "sinc"], writes=UK(2, 4))
                    tt("pool", out_ap, fsc(0), fsc(1), ALU.add, reads=UK(0, 4), writes=outkeys)

                if lvl < 2:
                    continue
                wb, wk = load_w(1)
                for m in range(4):
                    b = proj_fm(wb, wk, m)
                    rope(b, KA[:, m, p0:p0 + CH], [("KA", m, ci)])
                if lvl < 2.1:
                    continue
                for m in range(4):
                    S.op("dve", lambda e, m=m, p0=p0: e.tensor_reduce(
                        out=kmf[:, m, :], in_=KA[:, m, p0:p0 + CH].rearrange("p (a b) -> p a b", a=2),
                        axis=AX.X, op=ALU.add), reads=[("KA", m, ci)], writes=[("kmf", m)])
                    act(kmT[:, m, 2 * c:2 * c + 2], kmf[:, m, :], AF.Copy, reads=[("kmf", m)],
                        writes=[("kmT", m)], scale=1.0 / 256.0)
                if lvl < 2.2:
                    continue
                wb, wk = load_w(4)
                for m in range(4):
                    b = proj_fm(wb, wk, m)
                    rope(b, KB[:, m, p0:p0 + CH], [("KB", m, ci)])
                if lvl < 2.3:
                    continue
                wb, wk = load_w(7)
                b = proj_fm(wb, wk, 0)
                rope(b, KI[:, p0:p0 + CH], [("KI", ci)])
                if lvl < 2.4:
                    continue
                for j in range(4):
                    for kc in range(8):
                        mm(P[2][:, j * 8:(j + 1) * 8], hT[:, kc, j * 128:(j + 1) * 128],
                           wb[:, kc * 512 + 128: kc * 512 + 136], kc == 0, kc == 7,
                           reads=[wk, ("hT", j)], writes=[PK(2)])
                act(wis[:].rearrange("p a b -> p (a b)"), P[2][:, 0:32], AF.Copy, reads=[PK(2)], writes=["wis"])
                if lvl < 2.5:
                    continue
                for (g, Vt, vname) in ((2, VA, "VA"), (5, VB, "VB")):
                    wb, wk = load_w(g)
                    for j in range(4):
                        b = next_acc()
                        kt = 4 * c + j
                        for kc in range(8):
                            mm(P[b][:, :], hT[:, kc, j * 128:(j + 1) * 128], wb[:, kc * 512:(kc + 1) * 512],
                               kc == 0, kc == 7, reads=[wk, ("hT", j)], writes=[PK(b)])
                        pv = P[b][:, :].rearrange("p (a b c) -> p a b c", a=4, b=2)
                        act(Vt[:, kt, 0:12:3, :], pv[:, :, 0, :], AF.Copy, reads=[PK(b)], writes=[(vname, kt, 0)])
                        S.op("dve", lambda e, Vt=Vt, kt=kt, pv=pv: e.tensor_copy(out=Vt[:, kt, 2:12:3, :], in_=pv[:, :, 1, :]),
                             reads=[PK(b)], writes=[(vname, kt, 1)])
                if lvl < 2.6:
                    continue
                for (g, Qt, qname) in ((0, QA, "QA"), (3, QB, "QB"), (6, QI, "QI")):
                    wb, wk = load_w(g)
                    for m in range(4):
                        b = proj_fm(wb, wk, m)
                        rope(b, Qt[:, m, :], [(qname, m)])

                if lvl < 3:
                    continue
                if c >= 2:
                    for j in range(4):
                        npast = 2 * c + j // 2
                        for h in range(8):
                            p_, e_ = h // 2, h % 2
                            S.pe_drain()
                            mm(P[2][:, h * 8:h * 8 + npast], QA[64 * e_:64 * e_ + 64, p_, j * 128:(j + 1) * 128],
                               kmT[64 * e_:64 * e_ + 64, p_, 0:npast], True, True,
                               reads=[("QA", p_), ("kmT", p_)], writes=[PK(2)])
                        S.op("dve", lambda e: e.memset(gsb[:], -1.0e30), writes=["gsb"])
                        S.op("dve", lambda e, npast=npast: e.tensor_copy(
                            out=gsb[:, :, 0:npast], in_=P[2][:, 0:64].rearrange("p (a b) -> p a b", a=8)[:, :, 0:npast]),
                            reads=[PK(2)], writes=["gsb"])
                        for h in range(8):
                            S.op("dve", lambda e, h=h: e.max(out=gtop[:, h, :], in_=gsb[:, h, :]),
                                 reads=["gsb"], writes=[("gtop", h)])
                        tt("dve", gtmp[:, :, 0:npast], gsb[:, :, 0:npast],
                           gtop[:, :, 2:3].to_broadcast([128, 8, npast]), ALU.is_lt,
                           reads=["gsb"] + [("gtop", h) for h in range(8)], writes=["gtmp"])
                        for s2 in range(3):
                            ng = min(3, 8 - 3 * s2)
                            ts("dve", BT[:, s2, 0:ng, 0:npast], gtmp[:, 3 * s2:3 * s2 + ng, 0:npast], NEGB, None, ALU.mult, None,
                               reads=["gtmp"], writes=["BT"])
                        S.op("dve", lambda e, npast=npast: e.memset(BT[:, :, :, npast:8], 0.0), writes=["BT"])
                        for s2 in range(3):
                            tr(PB[:, s2 * 128:(s2 + 1) * 128], BT[:, s2, :, :].rearrange("p g n -> p (g n)"), ident_b,
                               reads=["BT", "cstb"], writes=[PBK])
                        act(BIAS[:, :, j * 128:(j + 1) * 128], PB[:, 0:384].rearrange("p (a b) -> p a b", a=3), AF.Copy,
                            reads=[PBK], writes=[("BIAS", j)])

                nkt = 4 * c + 4
                ptc = {"i": 0}

                def attn_head(branch, h):
                    Kt, Qt, Vt, Ot = (KA, QA, VA, OA) if branch == 0 else (KB, QB, VB, OB)
                    kn, qn, vn, on = ("KA", "QA", "VA", "OA") if branch == 0 else ("KB", "QB", "VB", "OB")
                    p_, e_ = h // 2, h % 2
                    ov = 5 + (h % 2)
                    Vf = Vt[:].rearrange("p k s d -> p k (s d)")
                    if e_ == 0:
                        lv = lambda kt: Vf[:, kt, 192 * p_:192 * p_ + 128]
                    else:
                        lv = lambda kt: Vf[:, kt, 192 * p_ + 64:192 * p_ + 192]
                    pend = [None]
                    for kt in range(nkt):
                        i0 = max(0, kt - 4 * c)
                        c0 = i0 * 128
                        sbk = 3 + (kt % 2)
                        kkey = (kn, p_, s_i * NCH_SEQ + kt // 4)
                        has_tri = kt >= 4 * c
                        has_bias = branch == 0 and c >= 2 and kt // 2 <= 2 * c
                        mm(P[sbk][:, c0:CH], Kt[64 * e_:64 * e_ + 64, p_, kt * 128:(kt + 1) * 128],
                           Qt[64 * e_:64 * e_ + 64, p_, c0:CH], True, not (has_tri or has_bias),
                           reads=[kkey, (qn, p_)], writes=[PK(sbk)])
                        if has_tri:
                            mm(P[sbk][:, c0:c0 + 128], ident_b, tri_b, False, not has_bias, reads=["cstb"], writes=[PK(sbk)])
                        if has_bias:
                            s2, g2 = h // 3, h % 3
                            S.pe_drain()
                            mm(P[sbk][:, c0:CH], indb[32 * g2:32 * g2 + 32, kt // 2, :],
                               BIAS[32 * g2:32 * g2 + 32, s2, c0:CH], False, True,
                               reads=["indb"] + [("BIAS", jj) for jj in range(4)], writes=[PK(sbk)])
                        pt_i = ptc["i"] % 3
                        ptc["i"] += 1
                        pT = PT[pt_i]
                        act(pT[:, c0:CH], P[sbk][:, c0:CH], AF.Exp, reads=[PK(sbk)], writes=[("pT", pt_i)], scale=0.125)
                        if branch == 1:
                            tt("dve", pT[:, c0:CH], pT[:, c0:CH], maskT(kt, c0, CH), ALU.mult,
                               reads=[("pT", pt_i), ("U", kt)], writes=[("pT", pt_i)])
                        if pend[0] is not None:
                            pend[0]()
                        pend[0] = (lambda kt=kt, c0=c0, pT=pT, pt_i=pt_i: mm(
                            P[ov][:, c0:CH], lv(kt), pT[:, c0:CH], kt == 0, kt == nkt - 1,
                            reads=[(vn, kt, e_), vn + "1", ("pT", pt_i)], writes=[PK(ov)]))
                    pend[0]()
                    if e_ == 0:
                        S.op("dve", lambda e, ov=ov: e.reciprocal(out=rden[0:64, :], in_=P[ov][64:128, :]),
                             reads=[PK(ov)], writes=[("rden", 0)])
                        tt("dve", Ot[0:64, p_, :], P[ov][0:64, :], rden[0:64, :], ALU.mult,
                           reads=[PK(ov), ("rden", 0)], writes=[(on, p_, 0)])
                    else:
                        S.op("dve", lambda e, ov=ov: e.reciprocal(out=rden[64:128, :], in_=P[ov][0:64, :]),
                             reads=[PK(ov)], writes=[("rden", 1)])
                        tt("dve", Ot[64:128, p_, :], P[ov][64:128, :], rden[64:128, :], ALU.mult,
                           reads=[PK(ov), ("rden", 1)], writes=[(on, p_, 1)])


                for j in range(4):
                    qt = 4 * c + j
                    ns = (qt + 1) * 128
                    for h in range(8):
                        ts("pool", Dw[:, h, :], ident_b, wis[:, j, h:h + 1], IDX_SCALE, ALU.mult, ALU.mult,
                           reads=["cstb", "wis"], writes=[("Dw", h)])
                    npc = (ns + 511) // 512
                    for pc in range(npc):
                        k0 = pc * 512
                        wd = min(512, ns - k0)
                        kik = [("KI", s_i * NCH_SEQ + pc)]
                        dpend = [None]
                        for h in range(8):
                            p_, e_ = h // 2, h % 2
                            b = next_acc()
                            mm(P[b][:, 0:wd], QI[64 * e_:64 * e_ + 64, p_, j * 128:(j + 1) * 128],
                               KI[64 * e_:64 * e_ + 64, k0:k0 + wd], True, True,
                               reads=[("QI", p_)] + kik, writes=[PK(b)])
                            act(RL[h % 2][:, 0:wd], P[b][:, 0:wd], AF.Relu, reads=[PK(b)], writes=[("RL", h % 2)])
                            if dpend[0] is not None:
                                dpend[0]()
                            dpend[0] = (lambda h=h, wd=wd: mm(P[2][:, 0:wd], Dw[:, h, :], RL[h % 2][:, 0:wd], h == 0, h == 7,
                                                               reads=[("Dw", h), ("RL", h % 2)], writes=[PK(2)]))
                        dpend[0]()
                        dpend[0] = None
                        if pc == npc - 1:
                            dcol = ns - 128 - k0
                            if dcol > 0:
                                act(Ibuf[:, k0:k0 + dcol], P[2][:, 0:dcol], AF.Copy, reads=[PK(2)], writes=[("I", pc)])
                            tt("dve", Ibuf[:, ns - 128:ns], P[2][:, dcol:dcol + 128], tri_tok, ALU.add,
                               reads=[PK(2), "cstf"], writes=[("I", 4)])
                        else:
                            act(Ibuf[:, k0:k0 + 512], P[2][:, :], AF.Copy, reads=[PK(2)], writes=[("I", pc)])
                    ik = [("I", i) for i in range(5)]
                    if qt >= 2:
                        S.op("dve", lambda e, ns=ns: e.tensor_reduce(out=st[:, 4:5], in_=Ibuf[:, 0:ns], axis=AX.X, op=ALU.max),
                             reads=ik, writes=[("st", 4)])
                        S.op("dve", lambda e, ns=ns: e.tensor_reduce(out=st[:, 5:6], in_=Ibuf[:, 0:ns - 128], axis=AX.X, op=ALU.min),
                             reads=ik, writes=[("st", 5)])
                        tt("dve", st[:, 6:7], st[:, 4:5], st[:, 5:6], ALU.subtract, reads=[("st", 4), ("st", 5)], writes=[("st", 6)])
                        ts("dve", stp[:], pow2[:], st[:, 6:7], None, ALU.mult, None, reads=["pow2", ("st", 6)], writes=["stp"])
                        stt(st[:, 7:8], st[:, 6:7], 0.5, st[:, 5:6], ALU.mult, ALU.add,
                            reads=[("st", 6), ("st", 5)], writes=[("st", 7)])
                        for it in range(NIT):
                            ts("dve", maskb[:, 0:ns], Ibuf[:, 0:ns], st[:, 7:8], None, ALU.is_ge, ALU.add,
                               reads=ik + [("st", 7)], writes=["maskb", ("st", 8)], accum_out=st[:, 8:9])
                            ts("dve", st[:, 9:10], st[:, 8:9], 255.5, 0.5, ALU.is_ge, ALU.subtract,
                               reads=[("st", 8)], writes=[("st", 9)])
                            stt(st[:, 7:8], st[:, 9:10], stp[:, it:it + 1], st[:, 7:8], ALU.mult, ALU.add,
                                reads=[("st", 9), "stp", ("st", 7)], writes=[("st", 7)])
                        ts("dve", maskb[:, 0:ns], Ibuf[:, 0:ns], st[:, 7:8], None, ALU.is_ge, None,
                           reads=ik + [("st", 7)], writes=["maskb"])
                    else:
                        ts("dve", maskb[:, 0:ns], Ibuf[:, 0:ns], -1.0e29, None, ALU.is_ge, None,
                           reads=ik, writes=["maskb"])
                    attn_head(0, 2 * j)
                    attn_head(0, 2 * j + 1)
                    for k8 in range(0, qt + 1, 8):
                        n8 = min(8, qt + 1 - k8)
                        for t8 in range(n8):
                            kt = k8 + t8
                            tr(PB[:, t8 * 128:(t8 + 1) * 128], maskb[:, kt * 128:(kt + 1) * 128], ident_b,
                               reads=["maskb", "cstb"], writes=[PBK])
                        for t8 in range(n8):
                            kt = k8 + t8
                            act(maskT(kt, j * 128, (j + 1) * 128), PB[:, t8 * 128:(t8 + 1) * 128], AF.Copy,
                                reads=[PBK], writes=[("U", kt)])

                for h in range(8):
                    attn_head(1, h)

                if lvl < 5:
                    continue
                wga = [load_w(8), None]
                oak = [("OA", p_, e_) for p_ in range(4) for e_ in range(2)]
                obk = [("OB", p_, e_) for p_ in range(4) for e_ in range(2)]
                SGA = Ibuf[:].bitcast(BF16)
                SGB = maskb[:]
                ikeys = [("I", i) for i in range(5)]
                for half in range(2):
                    wga_b, wga_k = wga[0] if half == 0 else load_w(9)
                    for mm_ in range(4):
                        m = half * 4 + mm_
                        b = proj_fm(wga_b, wga_k, mm_)
                        act(SGA[:, m * 512:(m + 1) * 512], P[b][:, :], AF.Sigmoid, reads=[PK(b)], writes=ikeys)
                wpa_b, wpa_k = load_w(12)
                for m in range(8):
                    b = next_acc()
                    for p_ in range(4):
                        mm(P[b][:, :], wpa_b[:, p_ * 1024 + m * 128: p_ * 1024 + (m + 1) * 128], OA[:, p_, :],
                           p_ == 0, p_ == 3, reads=[wpa_k] + oak, writes=[PK(b)])
                    tt("dve", fsc(0), P[b][:, :], SGA[:, m * 512:(m + 1) * 512], ALU.mult,
                       reads=[PK(b)] + ikeys, writes=UK(0, 2))
                    S.op("pool", lambda e, m=m: e.tensor_copy(out=SGA[:, m * 512:(m + 1) * 512], in_=fsc(0)),
                         reads=UK(0, 2) + ikeys, writes=ikeys)
                for half in range(2):
                    wgb_b, wgb_k = load_w(10 + half)
                    for mm_ in range(4):
                        b = proj_fm(wgb_b, wgb_k, mm_)
                        act(SGB[:, mm_ * 512:(mm_ + 1) * 512], P[b][:, :], AF.Sigmoid, reads=[PK(b)], writes=["maskb"])
                    wpb_b, wpb_k = load_w(13)
                    for mm_ in range(4):
                        m = half * 4 + mm_
                        b = next_acc()
                        for p_ in range(4):
                            mm(P[b][:, :], wpb_b[:, p_ * 1024 + m * 128: p_ * 1024 + (m + 1) * 128], OB[:, p_, :],
                               p_ == 0, p_ == 3, reads=[wpb_k] + obk, writes=[PK(b)])
                        tt("dve", fsc(1), P[b][:, :], SGB[:, mm_ * 512:(mm_ + 1) * 512], ALU.mult,
                           reads=[PK(b), "maskb"], writes=UK(2, 4))
                        tt("pool", mixT[:, m, :], fsc(1), SGA[:, m * 512:(m + 1) * 512], ALU.add,
                           reads=UK(2, 4) + ikeys, writes=[("QA", m) if m < 4 else ("QB", m - 4)])
                mixk = [("QA", m) for m in range(4)] + [("QB", m) for m in range(4)]

                if lvl < 6:
                    continue
                wo = [load_w(14), load_w(15)]
                for j in range(4):
                    xt = XT[j % 2]
                    xk = ("xt", j % 2)
                    r0 = tok0 + j * 128
                    tile_i = r0 // 128
                    S.dma("sp", lambda e, xt=xt, r0=r0: e.dma_start(out=xt[:], in_=x_d[r0:r0 + 128, :]), xk, writes=[xk])
                    for nh in range(2):
                        wob, wok = wo[nh]
                        ob_ = 5 + nh
                        for m in range(8):
                            mm(P[ob_][:, :], mixT[:, m, j * 128:(j + 1) * 128], wob[:, m * 512:(m + 1) * 512],
                               m == 0, m == 7, reads=[wok] + mixk, writes=[PK(ob_)])
                        tt("dve", xt[:, nh * 512:(nh + 1) * 512], P[ob_][:, :], xt[:, nh * 512:(nh + 1) * 512], ALU.add,
                           reads=[PK(ob_), xk], writes=[xk])
                    S.dma("sp", lambda e, xt=xt, r0=r0: e.dma_start(out=x1s_d[r0:r0 + 128, :], in_=xt[:]), ("x1st", j % 2),
                          reads=[xk], writes=[("x1s", tile_i)])
                    if not do_moe:
                        continue
                    rms_stats(xt[:], xk, 0)
                    stt(xnf, xt[:], st[:, 0:1], gffn[:], ALU.mult, ALU.mult, reads=[xk, ("st", 0), "gffn"], writes=UK(8, 12))
                    xq = xnb[j % 2]
                    xqk = ("xnb", 0)
                    act(xq[:], xnf, AF.Copy, reads=UK(8, 12), writes=[xqk])
                    for hf in range(2):
                        for k4 in range(4):
                            kc = hf * 4 + k4
                            tr(P[3 + hf][:, k4 * 128:(k4 + 1) * 128], xnf[:, kc * 128:(kc + 1) * 128], ident_f,
                               reads=UK(8, 12) + ["cstf"], writes=[PK(3 + hf)])
                        act(xnT[:, hf * 512:(hf + 1) * 512], P[3 + hf][:, :], AF.Copy, reads=[PK(3 + hf)],
                            writes=UK(12 + 2 * hf, 14 + 2 * hf))
                    for kc in range(8):
                        mm(P[2][:, 0:36], xnT[:, kc * 128:(kc + 1) * 128], wr[:, kc, :], kc == 0, kc == 7,
                           reads=UK(12, 16) + ["wr"], writes=[PK(2)])
                    R_ = lambda a, b_: rt[:, a:b_]
                    rk = lambda n: ("rt", n)
                    tt("dve", R_(0, 36), P[2][:, 0:36], brs[:], ALU.add, reads=[PK(2), "brs"], writes=[rk("lg")])
                    S.op("dve", lambda e: e.tensor_reduce(out=rt[:, 36:37], in_=rt[:, 0:4], axis=AX.X, op=ALU.max),
                         reads=[rk("lg")], writes=[rk("gmax")])
                    ts("dve", R_(40, 44), R_(0, 4), R_(36, 37), None, ALU.is_ge, None, reads=[rk("lg"), rk("gmax")], writes=[rk("goh")])
                    ts("dve", R_(37, 38), R_(36, 37), -1.0, None, ALU.mult, None, reads=[rk("gmax")], writes=[rk("ngmax")])
                    act(R_(44, 48), R_(0, 4), AF.Exp, reads=[rk("lg"), rk("ngmax")], writes=[rk("gexp"), rk("gsum")],
                        bias=rt[:, 37:38], accum_out=rt[:, 38:39])
                    S.op("dve", lambda e: e.reciprocal(out=rt[:, 39:40], in_=rt[:, 38:39]), reads=[rk("gsum")], writes=[rk("gw")])
                    ts("dve", R_(48, 52), R_(40, 44), 1.0, 1.0e30, ALU.subtract, ALU.mult, reads=[rk("goh")], writes=[rk("eb")])
                    tt("dve", rt[:, 64:96].rearrange("p (a b) -> p a b", a=4), rt[:, 4:36].rearrange("p (a b) -> p a b", a=4),
                       rt[:, 48:52].rearrange("p (a b) -> p a b", b=1).to_broadcast([128, 4, 8]), ALU.add,
                       reads=[rk("lg"), rk("eb")], writes=[rk("elm")])
                    S.op("dve", lambda e: e.max(out=rt[:, 96:104], in_=rt[:, 64:96]), reads=[rk("elm")], writes=[rk("top")])
                    ts("dve", R_(128, 160), R_(64, 96), R_(96, 97), None, ALU.is_equal, None, reads=[rk("elm"), rk("top")], writes=[rk("A1")])
                    ts("dve", R_(160, 192), R_(64, 96), R_(97, 98), None, ALU.is_equal, None, reads=[rk("elm"), rk("top")], writes=[rk("A2")])
                    tt("dve", R_(104, 105), R_(97, 98), R_(96, 97), ALU.subtract, reads=[rk("top")], writes=[rk("d21")])
                    act(R_(105, 106), R_(104, 105), AF.Exp, reads=[rk("d21")], writes=[rk("e21")])
                    ts("dve", R_(106, 107), R_(105, 106), 1.0, None, ALU.add, None, reads=[rk("e21")], writes=[rk("den")])
                    S.op("dve", lambda e: e.reciprocal(out=rt[:, 107:108], in_=rt[:, 106:107]), reads=[rk("den")], writes=[rk("rden")])
                    tt("dve", GATES[:, tile_i, 0:1], R_(39, 40), R_(107, 108), ALU.mult, reads=[rk("gw"), rk("rden")], writes=[("G", tile_i, 0)])
                    tt("dve", GATES[:, tile_i, 1:2], GATES[:, tile_i, 0:1], R_(105, 106), ALU.mult,
                       reads=[("G", tile_i, 0), rk("e21")], writes=[("G", tile_i, 1)])
                    tt("dve", a12[:], R_(128, 160), R_(160, 192), ALU.add, reads=[rk("A1"), rk("A2")], writes=["a12"])
                    mm(P[2][:, 64:96], su_b, a12[:], True, True, reads=["a12", "cstb"], writes=[PK(2)])
                    mm(P[2][:, 96:128], ones_b, a12[:], True, True, reads=["a12", "cstb"], writes=[PK(2)])
                    tt("dve", R_(192, 224), P[2][:, 64:96], basec[:], ALU.add, reads=[PK(2), "basec"], writes=[rk("pos")])
                    ts("dve", R_(224, 256), R_(192, 224), float(CAP), None, ALU.is_lt, None, reads=[rk("pos")], writes=[rk("ok")])
                    tt("dve", R_(192, 224), R_(192, 224), eoff[:], ALU.add, reads=[rk("pos"), "eoff"], writes=[rk("pos")])
                    stt(R_(192, 224), R_(192, 224), -BIGIDX, R_(224, 256), ALU.add, ALU.mult, reads=[rk("pos"), rk("ok")], writes=[rk("pos")])
                    ts("dve", R_(192, 224), R_(192, 224), BIGIDX, None, ALU.add, None, reads=[rk("pos")], writes=[rk("pos")])
                    stt(R_(64, 96), R_(128, 160), 1.0, R_(192, 224), ALU.mult, ALU.mult, reads=[rk("A1"), rk("pos"), rk("elm")],
                        writes=[rk("elm"), rk("d1")], accum_out=rt[:, 108:109])
                    stt(R_(64, 96), R_(160, 192), 1.0, R_(192, 224), ALU.mult, ALU.mult, reads=[rk("A2"), rk("pos"), rk("elm")],
                        writes=[rk("elm"), rk("d2")], accum_out=rt[:, 109:110])
                    S.op("dve", lambda e, tile_i=tile_i: e.tensor_copy(out=DEST[:, tile_i, :], in_=rt[:, 108:110]),
                         reads=[rk("d1"), rk("d2")], writes=[("DEST", tile_i)])
                    tt("dve", basec[:], P[2][:, 96:128], basec[:], ALU.add, reads=[PK(2), "basec"], writes=["basec"])
                    for k in range(2):
                        S.dma("pool", lambda e, tile_i=tile_i, k=k, xq=xq: e.indirect_dma_start(
                            out=xs_d[:, :], out_offset=bass.IndirectOffsetOnAxis(ap=DEST[:, tile_i, k:k + 1], axis=0),
                            in_=xq[:, :], in_offset=None, bounds_check=S.regs["bc"], oob_is_err=False),
                            ("sc", j % 2, k), reads=[xqk, ("DEST", tile_i)], writes=[("xs",)])
            S.barrier()
            S.emit(nc, stack)

        if do_moe and not skip2:
            ph2 = ExitStack()
            with ph2:
                def sb2(name, shape, dt):
                    return ph2.enter_context(nc.sbuf_tensor(name, shape, dt))
                stg = [sb2("stg%d" % i, [128, 4096], F32) for i in range(2)]
                Wb = [[sb2("W%d_%d" % (k, i), [128, 4096], BF16) for k in range(3)] for i in range(2)]
                xsb = [sb2("xsb%d" % i, [128, D], BF16) for i in range(2)]
                xT = sb2("xT", [128, 8, CAP], BF16)
                hidT = sb2("hidT", [128, 4, CAP], BF16)
                s1 = [sb2("s1_%d" % i, [128, 512], F32) for i in range(2)]
                yt = [sb2("yt%d" % i, [128, D], F32) for i in range(2)]
                NST = CAP // 128
                halves = [(0, 512), (512, CAP - 512)] if CAP > 512 else [(0, CAP)]
                sg = {"i": 0}
                for ex in range(NEXP):
                    wi_ = ex % 2
                    for k, wd_ in enumerate((w1_d, w3_d, w2_d)):
                        si = sg["i"] % 2
                        sg["i"] += 1
                        S.dma("sp", lambda e, wd_=wd_, ex=ex, si=si: e.dma_start(out=stg[si][:], in_=wd_[ex, :, :]),
                              ("stg", si), writes=[("stg", si)])
                        S.op("pool", lambda e, wi_=wi_, k=k, si=si: e.tensor_copy(out=Wb[wi_][k][:], in_=stg[si][:]),
                             reads=[("stg", si)], writes=[("W", wi_, k)])
                    W1, W3, W2 = Wb[wi_]
                    for sti in range(NST):
                        r0 = ex * CAP + sti * 128
                        xq = xsb[sti % 2]
                        S.dma("sp", lambda e, xq=xq, r0=r0: e.dma_start(out=xq[:], in_=xs_d[r0:r0 + 128, :]),
                              ("xsb", sti % 2), writes=[("xsb", sti % 2)])
                        for kc in range(8):
                            tr(PB[:, kc * 128:(kc + 1) * 128], xq[:, kc * 128:(kc + 1) * 128], ident_b,
                               reads=[("xsb", sti % 2), "cstb"], writes=[PBK])
                        S.op("dve", lambda e, sti=sti: e.tensor_copy(out=xT[:, :, sti * 128:(sti + 1) * 128],
                                                                     in_=PB[:, :].rearrange("p (a b) -> p a b", a=8)),
                             reads=[PBK], writes=[("xT", sti)])
                    xTk = [("xT", i) for i in range(NST)]
                    for f in range(4):
                        for hi_, (n0, w_) in enumerate(halves):
                            for kc in range(8):
                                mm(P[0][:, 0:w_], W1[:, kc * 512 + f * 128: kc * 512 + (f + 1) * 128], xT[:, kc, n0:n0 + w_],
                                   kc == 0, kc == 7, reads=[("W", wi_, 0)] + xTk, writes=[PK(0)])
                            for kc in range(8):
                                mm(P[1][:, 0:w_], W3[:, kc * 512 + f * 128: kc * 512 + (f + 1) * 128], xT[:, kc, n0:n0 + w_],
                                   kc == 0, kc == 7, reads=[("W", wi_, 1)] + xTk, writes=[PK(1)])
                            act(s1[hi_][:, 0:w_], P[0][:, 0:w_], AF.Silu, reads=[PK(0)], writes=[("s1", hi_)])
                            tt("dve", hidT[:, f, n0:n0 + w_], P[1][:, 0:w_], s1[hi_][:, 0:w_], ALU.mult,
                               reads=[PK(1), ("s1", hi_)], writes=[("hid", f, hi_)])
                    hk = [("hid", f, hi_) for f in range(4) for hi_ in range(len(halves))]
                    for sti in range(NST):
                        r0 = ex * CAP + sti * 128
                        yq = yt[sti % 2]
                        for nh in range(2):
                            pb_ = 3 + nh
                            for f in range(4):
                                mm(P[pb_][:, :], hidT[:, f, sti * 128:(sti + 1) * 128], W2[:, f * 1024 + nh * 512: f * 1024 + (nh + 1) * 512],
                                   f == 0, f == 3, reads=[("W", wi_, 2)] + hk, writes=[PK(pb_)])
                            if nh == 0:
                                act(yq[:, 0:512], P[pb_][:, :], AF.Copy, reads=[PK(pb_)], writes=[("yt", sti % 2)])
                            else:
                                S.op("dve", lambda e, yq=yq, pb_=pb_: e.tensor_copy(out=yq[:, 512:1024], in_=P[pb_][:, :]),
                                     reads=[PK(pb_)], writes=[("yt", sti % 2)])
                        S.dma("sp", lambda e, yq=yq, r0=r0: e.dma_start(out=ys_d[r0:r0 + 128, :], in_=yq[:]),
                              ("yts", sti % 2), reads=[("yt", sti % 2)], writes=[("ys",)])
                S.barrier()
                S.emit(nc, stack)

        ph3 = ExitStack()
        with ph3:
            def sb3(name, shape, dt):
                return ph3.enter_context(nc.sbuf_tensor(name, shape, dt))
            gfin = sb3("gfin", [128, D], F32)
            y1 = [sb3("y1_%d" % i, [128, D], F32) for i in range(2)]
            y2 = [sb3("y2_%d" % i, [128, D], F32) for i in range(2)]
            x1t = [sb3("x1t%d" % i, [128, D], F32) for i in range(2)]
            ot = [sb3("ot%d" % i, [128, D], F32) for i in range(2)]
            junk = sb3("junk", [128, D], BF16)
            st3 = sb3("st3", [128, 4], F32)
            S.dma("sp", lambda e: e.dma_start(out=gfin[:], in_=gv_d[2, :, :]), "gfin", writes=["gfin"])
            for ti in range(n_chunks * 4 if p3_tiles is None else p3_tiles):
                r0 = ti * 128
                k2 = ti % 2
                S.dma("sp", lambda e, k2=k2, r0=r0: e.dma_start(out=x1t[k2][:], in_=x1s_d[r0:r0 + 128, :]),
                      ("x1l", k2), writes=[("x1t", k2)])
                acc = x1t[k2]
                if do_moe:
                    for k, yy in enumerate((y1, y2)):
                        S.op("pool", lambda e, yy=yy, k2=k2: e.memset(yy[k2][:], 0.0), writes=[("y", k, k2)])
                        S.dma("pool", lambda e, yy=yy, k2=k2, ti=ti, k=k: e.indirect_dma_start(
                            out=yy[k2][:, :], out_offset=None, in_=ys_d[:, :],
                            in_offset=bass.IndirectOffsetOnAxis(ap=DEST[:, ti, k:k + 1], axis=0),
                            bounds_check=S.regs["bc"], oob_is_err=False),
                            ("ga", k, k2), reads=[("ys",)], writes=[("y", k, k2)])
                        S.op("dve", lambda e, yy=yy, k2=k2, ti=ti, k=k, acc=acc: e.scalar_tensor_tensor(
                            out=acc[:], in0=yy[k2][:], scalar=GATES[:, ti, k:k + 1], in1=acc[:], op0=ALU.mult, op1=ALU.add),
                            reads=[("y", k, k2), ("x1t", k2)], writes=[("x1t", k2)])
                S.op("act", lambda e, acc=acc: e.activation(out=junk[:], in_=acc[:], func=AF.Square, accum_out=st3[:, 0:1]),
                     reads=[("x1t", k2)], writes=["junk", ("st3", 0)])
                S.op("act", lambda e: e.activation(out=st3[:, 1:2], in_=st3[:, 0:1], func=AF.Ln, scale=1.0 / D, bias=epsb[:, 0:1]),
                     reads=[("st3", 0), "epsb"], writes=[("st3", 1)])
                S.op("act", lambda e: e.activation(out=st3[:, 0:1], in_=st3[:, 1:2], func=AF.Exp, scale=-0.5),
                     reads=[("st3", 1)], writes=[("st3", 0)])
                S.op("act", lambda e, acc=acc, k2=k2: e.activation(out=y1[k2][:], in_=acc[:], func=AF.Copy, scale=st3[:, 0:1]),
                     reads=[("x1t", k2), ("st3", 0)], writes=[("y", 0, k2)])
                S.op("pool", lambda e, k2=k2: e.tensor_tensor(out=ot[k2][:], in0=y1[k2][:], in1=gfin[:], op=ALU.mult),
                     reads=[("y", 0, k2), "gfin"], writes=[("ot", k2)])
                S.dma("sp", lambda e, k2=k2, r0=r0: e.dma_start(out=out_d[r0:r0 + 128, :], in_=ot[k2][:]),
                      ("ots", k2), reads=[("ot", k2)], writes=[("out", ti)])
            S.barrier()
            S.emit(nc, stack)
    return nc


def _host_consts():
    ident = np.eye(128, dtype=np.float32)
    perm = np.zeros((128, 128), np.float32)
    for m in range(128):
        if (m % 64) < 32:
            perm[m + 32, m] = -1.0
        else:
            perm[m - 32, m] = 1.0
    k = np.arange(128)[:, None]
    q = np.arange(128)[None, :]
    tri_kq = np.where(k <= q, 0.0, NEGB).astype(np.float32)
    su = (k < q).astype(np.float32)
    ones = np.ones((128, 128), np.float32)
    tri_tok = np.where(q <= k, 0.0, -1.0e30).astype(np.float32)
    cst = np.zeros((128, 8, 128), np.float32)
    for i, a in enumerate((ident, perm, tri_kq, su, ones, tri_tok)):
        cst[:, i, :] = a
    ind = np.zeros((128, 8, 128), np.float32)
    for g in range(4):
        for n in range(8):
            ind[32 * g + n, n, :] = 1.0
    inv = np.power(10000.0, -np.arange(0, 64, 2, dtype=np.float32) / 64).astype(np.float32)
    ang = np.arange(SEQ, dtype=np.float32)[:, None] * inv[None, :]
    cosr = np.tile(np.cos(ang).astype(np.float32).T, (4, 1))
    sinr = np.tile(np.sin(ang).astype(np.float32).T, (4, 1))
    eoff = np.tile((np.arange(32, dtype=np.float32) * CAP)[None, :], (128, 1))
    pow2 = np.tile((0.5 ** np.arange(1, NIT + 1, dtype=np.float32))[None, :], (128, 1)).astype(np.float32)
    return dict(cst=cst, ind=ind, cosr=np.ascontiguousarray(cosr), sinr=np.ascontiguousarray(sinr),
                eoff=eoff, pow2=pow2)


def _kc_layout(w):
    K, N = w.shape
    return np.ascontiguousarray(w.reshape(K // 128, 128, N).transpose(1, 0, 2).reshape(128, (K // 128) * N))


def _host_weights(inp):
    w_in = inp["w_in"][0]
    sp = np.cumsum([0, 512, 512, 512, 512, 512, 512, 512, 64, 8, 1024, 1024])
    qa, ka, va, qb, kb, vb, qi, ki, wi, ga, gb = [w_in[:, sp[i]:sp[i + 1]] for i in range(11)]
    g7 = np.zeros((1024, 512), np.float32)
    g7[:, 0:64] = ki
    g7[:, 64:128] = ki
    g7[:, 128:136] = wi
    groups = [qa, ka, va, qb, kb, vb, qi, g7, ga[:, :512], ga[:, 512:], gb[:, :512], gb[:, 512:]]
    wcat = np.zeros((16, 128, 4096), np.float32)
    for i, g in enumerate(groups):
        wcat[i] = _kc_layout(g)
    wcat[12] = _kc_layout(inp["w_proj_a"][0])
    wcat[13] = _kc_layout(inp["w_proj_b"][0])
    wo = inp["w_out"][0]
    wcat[14] = _kc_layout(wo[:, :512])
    wcat[15] = _kc_layout(wo[:, 512:])
    w1r = np.stack([_kc_layout(inp["w1"][0, e]) for e in range(NEXP)])
    w3r = np.stack([_kc_layout(inp["w3"][0, e]) for e in range(NEXP)])
    w2r = np.stack([_kc_layout(inp["w2"][0, e]) for e in range(NEXP)])
    wrc = np.concatenate([inp["w_group"][0], inp["w_expert"][0]], axis=1)
    wr = _kc_layout(wrc)
    br = np.tile(np.concatenate([inp["b_group"][0], inp["b_expert"][0]])[None, :], (128, 1)).astype(np.float32)
    gv = np.stack([np.tile(inp["g_mix"][0][None, :], (128, 1)), np.tile(inp["g_ffn"][0][None, :], (128, 1)),
                   np.tile(inp["g_final"][None, :], (128, 1))]).astype(np.float32)
    return dict(wcat=wcat, w1r=w1r, w3r=w3r, w2r=w2r, wr=wr, br=br, gv=gv)


_NC_CACHE = {}


def kernel(**inputs):
    inp = {k: np.asarray(v, dtype=np.float32) for k, v in inputs.items()}
    shared = _host_consts()
    shared.update(_host_weights(inp))
    x = inp["x"].reshape(NCORE, NT, D)
    if "nc" not in _NC_CACHE:
        _NC_CACHE["nc"] = build()
    nc = _NC_CACHE["nc"]
    in_maps = []
    for i in range(NCORE):
        m = dict(shared)
        m["x"] = np.ascontiguousarray(x[i])
        in_maps.append(m)
    res = run_bass_kernel_spmd(nc, in_maps, core_ids=list(range(NCORE)))
    out = np.concatenate([np.asarray(r["out"]) for r in res.results], axis=0)
    return out.reshape(32, SEQ, D).astype(np.float32)
```

```python
import os
from contextlib import ExitStack
import numpy as np
import concourse.bass as bass
import concourse.mybir as mybir
from concourse.bass_utils import run_bass_kernel_spmd

F32 = mybir.dt.float32
BF16 = mybir.dt.bfloat16
I32 = mybir.dt.int32
ALU = mybir.AluOpType
AF = mybir.ActivationFunctionType
AX = mybir.AxisListType

NCORE = 8
SEQ = 2048
D = 1024
SPC = 4
NT = SPC * SEQ
CH = 512
NCH_SEQ = SEQ // CH
NEXP = 32
CAP = 768
NEGB = -30000.0
NIT = 13
IDX_SCALE = float((8 * 64) ** -0.5)
EPS = 1e-6
BIGIDX = 1.0e6

ENGS = ("pe", "act", "dve", "pool", "sp")


class Sched:
    EP = 30000

    def __init__(self):
        self.q = {e: [] for e in ENGS}
        self.cnt = {e: 0 for e in ENGS}
        self.seen = {e: {} for e in ENGS}
        self.w = {}
        self.r = {}
        self.dma_cnt = {}
        self.dma_last = {}
        self.regs = {}

    def _deps(self, reads, writes):
        deps = {}

        def add(t):
            if t is not None and deps.get(t[0], 0) < t[1]:
                deps[t[0]] = t[1]
        for k in reads:
            add(self.w.get(k))
        for k in writes:
            add(self.w.get(k))
            for sk, v in self.r.get(k, {}).items():
                add((sk, v))
        return deps

    def _waits(self, eng, deps):
        for sk, v in deps.items():
            if eng == "pe" and sk == "pe":
                continue
            if self.seen[eng].get(sk, 0) >= v:
                continue
            self.seen[eng][sk] = v
            self.q[eng].append(("wait", sk, v))

    def _commit(self, tok, reads, writes):
        for k in reads:
            d = self.r.setdefault(k, {})
            if d.get(tok[0], 0) < tok[1]:
                d[tok[0]] = tok[1]
        for k in writes:
            self.w[k] = tok
            self.r[k] = {}

    def pe_drain(self):
        if self.cnt["pe"] > 0 and self.seen["pe"].get("pe", 0) < self.cnt["pe"]:
            self.seen["pe"]["pe"] = self.cnt["pe"]
            self.q["pe"].append(("wait", "pe", self.cnt["pe"]))

    def op(self, eng, fn, reads=(), writes=()):
        psr = [k for k in reads if isinstance(k, tuple) and k[0] in ("ps", "psb")]
        if psr:
            reads = [k for k in reads if k not in psr]
            writes = list(writes) + psr
        self._waits(eng, self._deps(reads, writes))
        self.cnt[eng] += 1
        tok = (eng, self.cnt[eng])
        self.q[eng].append(("op", fn, tok))
        self._commit(tok, reads, writes)

    def dma(self, eng, fn, key, reads=(), writes=()):
        deps = self._deps(reads, writes)
        sk = ("dma", key)
        if key in self.dma_last:
            t = self.dma_last[key]
            if deps.get(t[0], 0) < t[1]:
                deps[t[0]] = t[1]
        self._waits(eng, deps)
        self.dma_cnt[key] = self.dma_cnt.get(key, 0) + 16
        tok = (sk, self.dma_cnt[key])
        self.dma_last[key] = tok
        self.q[eng].append(("dma", fn, tok))
        self._commit(tok, reads, writes)

    def barrier(self):
        deps = {}
        for e in ("pe", "act", "dve", "pool"):
            if self.cnt[e] > 0:
                deps[e] = self.cnt[e]
        for key, c in self.dma_cnt.items():
            deps[("dma", key)] = c
        for e in ENGS:
            self._waits(e, dict(deps))

    def emit(self, nc, stack):
        if not hasattr(self, "sems"):
            self.sems = {}
        sems = self.sems

        def sem_of(sk, v):
            if isinstance(sk, tuple):
                if sk not in sems:
                    sems[sk] = stack.enter_context(nc.semaphore("d%d" % len(sems)))
                return sems[sk], v
            ep = (v - 1) // self.EP
            k2 = (sk, ep)
            if k2 not in sems:
                sems[k2] = stack.enter_context(nc.semaphore("c%d" % len(sems)))
            return sems[k2], (v - 1) % self.EP + 1

        for e in ENGS:
            for it in self.q[e]:
                if it[0] == "wait":
                    sem_of(it[1], it[2])
                else:
                    sem_of(it[2][0], it[2][1])
        print("[kernel] block instr counts", {e: len(self.q[e]) for e in ENGS}, "sems", len(sems), flush=True)
        with nc.Block() as block:
            def run(engname):
                def body(e):
                    if engname == "pool":
                        self.regs["bc"] = e.to_reg(NEXP * CAP - 1)
                    for ii, it in enumerate(self.q[engname]):
                      try:
                        if it[0] == "wait":
                            s, v = sem_of(it[1], it[2])
                            e.wait_ge(s, v)
                        elif it[0] == "op":
                            s, _ = sem_of(it[2][0], it[2][1])
                            it[1](e).then_inc(s, 1)
                        else:
                            s, _ = sem_of(it[2][0], it[2][1])
                            it[1](e).then_inc(s, 16)
                      except Exception:
                        print("[kernel] emit failure at", engname, ii, it[0], it[2] if it[0] != "wait" else it[1:], flush=True)
                        print([ (x[0], x[2] if x[0] != "wait" else x[1:]) for x in self.q[engname][max(0, ii - 6):ii]], flush=True)
                        raise
                return body
            block.tensor(run("pe"))
            block.scalar(run("act"))
            block.vector(run("dve"))
            block.gpsimd(run("pool"))
            block.sync(run("sp"))
        for e in ENGS:
            self.q[e] = []
        return len(sems)


def build(n_chunks=SPC * NCH_SEQ, do_moe=True, dbg=False, p3_tiles=None, skip2=False, lvl=99):
    nc = bass.Bass("TRN2", target_bir_lowering=False)
    S = Sched()

    def din(name, shape, dt=F32):
        return nc.dram_tensor(name, shape, dt, kind="ExternalInput").ap()

    x_d = din("x", [NT, D])
    wcat_d = din("wcat", [16, 128, 4096])
    nexp_in = NEXP if do_moe else 1
    w1_d = din("w1r", [nexp_in, 128, 4096])
    w3_d = din("w3r", [nexp_in, 128, 4096])
    w2_d = din("w2r", [nexp_in, 128, 4096])
    wr_d = din("wr", [128, 8 * 36])
    br_d = din("br", [128, 36])
    gv_d = din("gv", [3, 128, D])
    cos_d = din("cosr", [128, SEQ])
    sin_d = din("sinr", [128, SEQ])
    cst_d = din("cst", [128, 8, 128])
    ind_d = din("ind", [128, 8, 128])
    eoff_d = din("eoff", [128, 32])
    pow2_d = din("pow2", [128, NIT])
    out_d = nc.dram_tensor("out", [NT, D], F32, kind="ExternalOutput").ap()
    wsc_d = nc.dram_tensor("wsc", [16, 128, 4096], BF16, kind="Internal").ap()
    x1s_d = nc.dram_tensor("x1s", [NT, D], F32, kind=("ExternalOutput" if dbg else "Internal")).ap()
    xs_d = nc.dram_tensor("xs", [NEXP * CAP, D], BF16, kind="Internal").ap()
    ys_d = nc.dram_tensor("ys", [NEXP * CAP, D], F32, kind="Internal").ap()

    stack = ExitStack()
    with stack:
        def sb(name, shape, dt):
            return stack.enter_context(nc.sbuf_tensor(name, shape, dt))

        def pst(name, shape, dt):
            return stack.enter_context(nc.psum_tensor(name, shape, dt))

        P = [pst("ps%d" % i, [128, 512], F32) for i in range(7)]
        PB = pst("psb", [128, 1024], BF16)

        def PK(i):
            return ("ps", i)
        PBK = ("psb",)

        cstf = sb("cstf", [128, 8, 128], F32)
        cstb = sb("cstb", [128, 5, 128], BF16)
        indb = sb("indb", [128, 8, 128], BF16)
        eoff = sb("eoff_sb", [128, 32], F32)
        pow2 = sb("pow2_sb", [128, NIT], F32)
        wr = sb("wr_sb", [128, 8, 36], F32)
        brs = sb("br_sb", [128, 36], F32)
        epsb = sb("epsb", [128, 1], F32)
        GATES = sb("gates", [128, NT // 128, 2], F32)
        DEST = sb("dest", [128, NT // 128, 2], I32)
        basec = sb("basec", [128, 32], F32)
        ident_b = cstb[:, 0, :]
        perm_b = cstb[:, 1, :]
        tri_b = cstb[:, 2, :]
        su_b = cstb[:, 3, :]
        ones_b = cstb[:, 4, :]
        ident_f = cstf[:, 0, :]
        tri_tok = cstf[:, 5, :]

        S.dma("sp", lambda e: e.dma_start(out=cstf[:], in_=cst_d[:, :, :]), "cstf", writes=["cstf"])
        S.dma("sp", lambda e: e.dma_start(out=eoff[:], in_=eoff_d[:, :]), "eoff", writes=["eoff"])
        S.dma("sp", lambda e: e.dma_start(out=pow2[:], in_=pow2_d[:, :]), "pow2", writes=["pow2"])
        S.dma("sp", lambda e: e.dma_start(out=wr[:].rearrange("p a b -> p (a b)"), in_=wr_d[:, :]), "wr", writes=["wr"])
        S.dma("sp", lambda e: e.dma_start(out=brs[:], in_=br_d[:, :]), "brs", writes=["brs"])
        S.op("dve", lambda e: e.tensor_copy(out=cstb[:], in_=cstf[:, 0:5, :]), reads=["cstf"], writes=["cstb"])
        S.op("dve", lambda e: e.memset(epsb[:], EPS), writes=["epsb"])
        S.op("dve", lambda e: e.memset(basec[:], 0.0), writes=["basec"])

        ph1 = ExitStack()
        with ph1:
            def sb1(name, shape, dt):
                return ph1.enter_context(nc.sbuf_tensor(name, shape, dt))
            KA = sb1("KA", [128, 4, SEQ], BF16)
            KB = sb1("KB", [128, 4, SEQ], BF16)
            KI = sb1("KI", [128, SEQ], BF16)
            VA = sb1("VA", [128, 16, 12, 64], BF16)
            VB = sb1("VB", [128, 16, 12, 64], BF16)
            kmT = sb1("kmT", [128, 4, 8], BF16)
            kmf = sb1("kmf", [128, 4, 2], F32)
            hT = sb1("hT", [128, 8, CH], BF16)
            QAB = sb1("QAB", [128, 8, CH], BF16)
            QA = QAB[:, 0:4, :]
            QB = QAB[:, 4:8, :]
            mixT = QAB
            QI = sb1("QI", [128, 4, CH], BF16)
            rden = sb1("rden", [128, CH], F32)
            OA = sb1("OA", [128, 4, CH], BF16)
            OB = sb1("OB", [128, 4, CH], BF16)
            wbuf = [sb1("wbuf%d" % i, [128, 4096], BF16) for i in range(2)]
            XT = [sb1("xt%d" % i, [128, D], F32) for i in range(2)]
            hn = sb1("hn", [128, D], BF16)
            xnb = [sb1("xnb0", [128, D], BF16)] * 2
            gmix = sb1("gmix", [128, D], F32)
            gffn = sb1("gffn", [128, D], F32)
            cosc = sb1("cosc", [128, CH], F32)
            sinc = sb1("sinc", [128, CH], F32)
            xb = sb1("xb", [128, CH], BF16)
            PT = [sb1("pT%d" % i, [128, CH], BF16) for i in range(3)]
            RL = [sb1("RL%d" % i, [128, CH], BF16) for i in range(2)]
            Dw = sb1("Dw", [128, 8, 128], BF16)
            Ibuf = sb1("Ibuf", [128, SEQ], F32)
            maskb = sb1("maskb", [128, SEQ], BF16)
            wis = sb1("wis", [128, 4, 8], F32)
            st = sb1("st", [128, 16], F32)
            stp = sb1("stp", [128, NIT], F32)
            gsb = sb1("gsb", [128, 8, 8], F32)
            gtop = sb1("gtop", [128, 8, 8], F32)
            gtmp = sb1("gtmp", [128, 8, 8], F32)
            BT = sb1("BT", [128, 3, 4, 32], BF16)
            BIAS = sb1("BIAS", [128, 3, CH], BF16)
            rt = sb1("rt", [128, 256], F32)
            a12 = sb1("a12", [128, 32], BF16)
            U = sb1("U", [128, 4096], F32)
            Ub = U[:].bitcast(BF16)

            def UK(lo, hi):
                return [("U", i) for i in range(lo, hi)]

            def maskT(kt, c0, c1):
                return Ub[:, kt * 512 + c0: kt * 512 + c1]

            def fsc(i):
                return U[:, i * 512:(i + 1) * 512]
            xnf = U[:, 2048:3072]
            xnT = U[:, 3072:4096]

            S.dma("sp", lambda e: e.dma_start(out=gmix[:], in_=gv_d[0, :, :]), "gmix", writes=["gmix"])
            S.dma("sp", lambda e: e.dma_start(out=gffn[:], in_=gv_d[1, :, :]), "gffn", writes=["gffn"])
            S.dma("sp", lambda e: e.dma_start(out=U[:, 0:1024].rearrange("p (a b) -> p a b", a=8), in_=ind_d[:, :, :]),
                  "U0", writes=UK(0, 4))
            S.op("dve", lambda e: e.tensor_copy(out=indb[:], in_=U[:, 0:1024].rearrange("p (a b) -> p a b", a=8)),
                 reads=UK(0, 4), writes=["indb"])
            S.op("pool", lambda e: e.memset(VA[:, :, 1:12:3, :], 1.0), writes=["VA1"])
            S.op("pool", lambda e: e.memset(VB[:, :, 1:12:3, :], 1.0), writes=["VB1"])
            S.op("pool", lambda e: e.memset(BT[:], 0.0), writes=["BT"])
            for g in range(16):
                wb = wbuf[g % 2]
                S.dma("sp", lambda e, g=g: e.dma_start(out=U[:], in_=wcat_d[g, :, :]), "U0",
                      writes=UK(0, 16))
                eng = ("act", "dve", "pool")[g % 3]
                if eng == "act":
                    S.op("act", lambda e, wb=wb: e.activation(out=wb[:], in_=U[:], func=AF.Copy),
                         reads=UK(0, 16), writes=[("wb", g % 2)])
                else:
                    S.op(eng, lambda e, wb=wb: e.tensor_copy(out=wb[:], in_=U[:]),
                         reads=UK(0, 16), writes=[("wb", g % 2)])
                S.dma("sp", lambda e, g=g, wb=wb: e.dma_start(out=wsc_d[g, :, :], in_=wb[:]), ("wbs", g % 2),
                      reads=[("wb", g % 2)], writes=[("wsc", g)])

            S.op("pool", lambda e: e.memset(U[:], 0.0), reads=UK(0, 16), writes=UK(0, 16))
            for zi in range(NEXP * CAP // 1024):
                S.dma("sp", lambda e, zi=zi: e.dma_start(
                    out=xs_d[zi * 1024:(zi + 1) * 1024, :].rearrange("(a p) d -> p a d", p=128),
                    in_=Ub[:, :].rearrange("p (a d) -> p a d", a=8)), ("xsz", zi % 4), reads=UK(0, 16), writes=[("xs",)])
            wstate = {"i": 0}

            def load_w(g):
                k = wstate["i"] % 2
                wstate["i"] += 1
                wb = wbuf[k]
                S.dma("sp", lambda e: e.dma_start(out=wb[:], in_=wsc_d[g, :, :]), ("wbl", k),
                      reads=[("wsc", g)], writes=[("wb", k)])
                return wb, ("wb", k)

            def mm(out, lhsT, rhs, start, stop, reads, writes):
                S.op("pe", lambda e: e.matmul(out, lhsT=lhsT, rhs=rhs, start=start, stop=stop),
                     reads=reads, writes=writes)

            def tr(out, in_, ident, reads, writes):
                S.op("pe", lambda e: e.transpose(out, in_, ident), reads=reads, writes=writes)

            def act(out, in_, func, reads, writes, **kw):
                S.op("act", lambda e: e.activation(out=out, in_=in_, func=func, **kw), reads=reads, writes=writes)

            def tt(eng, out, in0, in1, op, reads, writes):
                S.op(eng, lambda e: e.tensor_tensor(out=out, in0=in0, in1=in1, op=op), reads=reads, writes=writes)

            def ts(eng, out, in0, s1, s2, op0, op1, reads, writes, **kw):
                if op1 is None:
                    S.op(eng, lambda e: e.tensor_scalar(out=out, in0=in0, scalar1=s1, scalar2=None, op0=op0, **kw),
                         reads=reads, writes=writes)
                else:
                    S.op(eng, lambda e: e.tensor_scalar(out=out, in0=in0, scalar1=s1, scalar2=s2, op0=op0, op1=op1, **kw),
                         reads=reads, writes=writes)

            def stt(out, in0, scalar, in1, op0, op1, reads, writes, **kw):
                S.op("dve", lambda e: e.scalar_tensor_tensor(out=out, in0=in0, scalar=scalar, in1=in1, op0=op0, op1=op1, **kw),
                     reads=reads, writes=writes)

            def rms_stats(src, srckey, col):
                stt(hn[:], src, 1.0, src, ALU.mult, ALU.mult, reads=[srckey], writes=["hn", ("st", col)],
                    accum_out=st[:, col:col + 1])
                act(st[:, col + 1:col + 2], st[:, col:col + 1], AF.Ln, reads=[("st", col), "epsb"],
                    writes=[("st", col + 1)], scale=1.0 / D, bias=epsb[:, 0:1])
                act(st[:, col:col + 1], st[:, col + 1:col + 2], AF.Exp, reads=[("st", col + 1)],
                    writes=[("st", col)], scale=-0.5)

            for ci in range(n_chunks):
                s_i = ci // NCH_SEQ
                c = ci % NCH_SEQ
                tok0 = ci * CH
                p0 = c * CH
                S.dma("sp", lambda e, p0=p0: e.dma_start(out=cosc[:], in_=cos_d[:, p0:p0 + CH]), "cosc", writes=["cosc"])
                S.dma("sp", lambda e, p0=p0: e.dma_start(out=sinc[:], in_=sin_d[:, p0:p0 + CH]), "sinc", writes=["sinc"])
                if lvl < 1:
                    continue
                for j in range(4):
                    xt = XT[j % 2]
                    xk = ("xt", j % 2)
                    r0 = tok0 + j * 128
                    S.dma("sp", lambda e, xt=xt, r0=r0: e.dma_start(out=xt[:], in_=x_d[r0:r0 + 128, :]), xk, writes=[xk])
                    rms_stats(xt[:], xk, 0)
                    stt(hn[:], xt[:], st[:, 0:1], gmix[:], ALU.mult, ALU.mult, reads=[xk, ("st", 0), "gmix"], writes=["hn"])
                    for kc in range(8):
                        tr(PB[:, kc * 128:(kc + 1) * 128], hn[:, kc * 128:(kc + 1) * 128], ident_b,
                           reads=["hn", "cstb"], writes=[PBK])
                    act(hT[:, :, j * 128:(j + 1) * 128], PB[:, :].rearrange("p (a b) -> p a b", a=8), AF.Copy,
                        reads=[PBK], writes=[("hT", j)])
                hTk = [("hT", j) for j in range(4)]

                accb = {"i": 0}

                def next_acc():
                    accb["i"] ^= 1
                    return accb["i"]

                def proj_fm(wb, wk, m):
                    b = next_acc()
                    for kc in range(8):
                        mm(P[b][:, :], wb[:, kc * 512 + m * 128: kc * 512 + (m + 1) * 128], hT[:, kc, :],
                           kc == 0, kc == 7, reads=[wk] + hTk, writes=[PK(b)])
                    return b

                def rope(b, out_ap, outkeys):
                    act(xb[:], P[b][:, :], AF.Copy, reads=[PK(b)], writes=["xb"])
                    mm(P[2][:, :], perm_b, xb[:], True, True, reads=["xb", "cstb"], writes=[PK(2)])
                    tt("dve", fsc(0), P[b][:, :], cosc[:], ALU.mult, reads=[PK(b), "cosc"], writes=UK(0, 2))
                    tt("dve", fsc(1), P[2][:, :], sinc[:], ALU.mult, reads=[PK(2), "sinc"], writes=UK(2, 4))
                    tt("pool", out_ap, fsc(0), fsc(1), ALU.add, reads=UK(0, 4), writes=outkeys)

                if lvl < 2:
                    continue
                wb, wk = load_w(1)
                for m in range(4):
                    b = proj_fm(wb, wk, m)
                    rope(b, KA[:, m, p0:p0 + CH], [("KA", m, ci)])
                if lvl < 2.1:
                    continue
                for m in range(4):
                    S.op("dve", lambda e, m=m, p0=p0: e.tensor_reduce(
                        out=kmf[:, m, :], in_=KA[:, m, p0:p0 + CH].rearrange("p (a b) -> p a b", a=2),
                        axis=AX.X, op=ALU.add), reads=[("KA", m, ci)], writes=[("kmf", m)])
                    act(kmT[:, m, 2 * c:2 * c + 2], kmf[:, m, :], AF.Copy, reads=[("kmf", m)],
                        writes=[("kmT", m)], scale=1.0 / 256.0)
                if lvl < 2.2:
                    continue
                wb, wk = load_w(4)
                for m in range(4):
                    b = proj_fm(wb, wk, m)
                    rope(b, KB[:, m, p0:p0 + CH], [("KB", m, ci)])
                if lvl < 2.3:
                    continue
                wb, wk = load_w(7)
                b = proj_fm(wb, wk, 0)
                rope(b, KI[:, p0:p0 + CH], [("KI", ci)])
                if lvl < 2.4:
                    continue
                for j in range(4):
                    for kc in range(8):
                        mm(P[2][:, j * 8:(j + 1) * 8], hT[:, kc, j * 128:(j + 1) * 128],
                           wb[:, kc * 512 + 128: kc * 512 + 136], kc == 0, kc == 7,
                           reads=[wk, ("hT", j)], writes=[PK(2)])
                act(wis[:].rearrange("p a b -> p (a b)"), P[2][:, 0:32], AF.Copy, reads=[PK(2)], writes=["wis"])
                if lvl < 2.5:
                    continue
                for (g, Vt, vname) in ((2, VA, "VA"), (5, VB, "VB")):
                    wb, wk = load_w(g)
                    for j in range(4):
                        b = next_acc()
                        kt = 4 * c + j
                        for kc in range(8):
                            mm(P[b][:, :], hT[:, kc, j * 128:(j + 1) * 128], wb[:, kc * 512:(kc + 1) * 512],
                               kc == 0, kc == 7, reads=[wk, ("hT", j)], writes=[PK(b)])
                        pv = P[b][:, :].rearrange("p (a b c) -> p a b c", a=4, b=2)
                        act(Vt[:, kt, 0:12:3, :], pv[:, :, 0, :], AF.Copy, reads=[PK(b)], writes=[(vname, kt, 0)])
                        S.op("dve", lambda e, Vt=Vt, kt=kt, pv=pv: e.tensor_copy(out=Vt[:, kt, 2:12:3, :], in_=pv[:, :, 1, :]),
                             reads=[PK(b)], writes=[(vname, kt, 1)])
                if lvl < 2.6:
                    continue
                for (g, Qt, qname) in ((0, QA, "QA"), (3, QB, "QB"), (6, QI, "QI")):
                    wb, wk = load_w(g)
                    for m in range(4):
                        b = proj_fm(wb, wk, m)
                        rope(b, Qt[:, m, :], [(qname, m)])

                if lvl < 3:
                    continue
                if c >= 2:
                    for j in range(4):
                        npast = 2 * c + j // 2
                        for h in range(8):
                            p_, e_ = h // 2, h % 2
                            S.pe_drain()
                            mm(P[2][:, h * 8:h * 8 + npast], QA[64 * e_:64 * e_ + 64, p_, j * 128:(j + 1) * 128],
                               kmT[64 * e_:64 * e_ + 64, p_, 0:npast], True, True,
                               reads=[("QA", p_), ("kmT", p_)], writes=[PK(2)])
                        S.op("dve", lambda e: e.memset(gsb[:], -1.0e30), writes=["gsb"])
                        S.op("dve", lambda e, npast=npast: e.tensor_copy(
                            out=gsb[:, :, 0:npast], in_=P[2][:, 0:64].rearrange("p (a b) -> p a b", a=8)[:, :, 0:npast]),
                            reads=[PK(2)], writes=["gsb"])
                        for h in range(8):
                            S.op("dve", lambda e, h=h: e.max(out=gtop[:, h, :], in_=gsb[:, h, :]),
                                 reads=["gsb"], writes=[("gtop", h)])
                        tt("dve", gtmp[:, :, 0:npast], gsb[:, :, 0:npast],
                           gtop[:, :, 2:3].to_broadcast([128, 8, npast]), ALU.is_lt,
                           reads=["gsb"] + [("gtop", h) for h in range(8)], writes=["gtmp"])
                        for s2 in range(3):
                            ng = min(3, 8 - 3 * s2)
                            ts("dve", BT[:, s2, 0:ng, 0:npast], gtmp[:, 3 * s2:3 * s2 + ng, 0:npast], NEGB, None, ALU.mult, None,
                               reads=["gtmp"], writes=["BT"])
                        S.op("dve", lambda e, npast=npast: e.memset(BT[:, :, :, npast:8], 0.0), writes=["BT"])
                        for s2 in range(3):
                            tr(PB[:, s2 * 128:(s2 + 1) * 128], BT[:, s2, :, :].rearrange("p g n -> p (g n)"), ident_b,
                               reads=["BT", "cstb"], writes=[PBK])
                        act(BIAS[:, :, j * 128:(j + 1) * 128], PB[:, 0:384].rearrange("p (a b) -> p a b", a=3), AF.Copy,
                            reads=[PBK], writes=[("BIAS", j)])

                nkt = 4 * c + 4
                ptc = {"i": 0}

                def attn_head(branch, h):
                    Kt, Qt, Vt, Ot = (KA, QA, VA, OA) if branch == 0 else (KB, QB, VB, OB)
                    kn, qn, vn, on = ("KA", "QA", "VA", "OA") if branch == 0 else ("KB", "QB", "VB", "OB")
                    p_, e_ = h // 2, h % 2
                    ov = 5 + (h % 2)
                    Vf = Vt[:].rearrange("p k s d -> p k (s d)")
                    if e_ == 0:
                        lv = lambda kt: Vf[:, kt, 192 * p_:192 * p_ + 128]
                    else:
                        lv = lambda kt: Vf[:, kt, 192 * p_ + 64:192 * p_ + 192]
                    pend = [None]
                    for kt in range(nkt):
                        i0 = max(0, kt - 4 * c)
                        c0 = i0 * 128
                        sbk = 3 + (kt % 2)
                        kkey = (kn, p_, s_i * NCH_SEQ + kt // 4)
                        has_tri = kt >= 4 * c
                        has_bias = branch == 0 and c >= 2 and kt // 2 <= 2 * c
                        mm(P[sbk][:, c0:CH], Kt[64 * e_:64 * e_ + 64, p_, kt * 128:(kt + 1) * 128],
                           Qt[64 * e_:64 * e_ + 64, p_, c0:CH], True, not (has_tri or has_bias),
                           reads=[kkey, (qn, p_)], writes=[PK(sbk)])
                        if has_tri:
                            mm(P[sbk][:, c0:c0 + 128], ident_b, tri_b, False, not has_bias, reads=["cstb"], writes=[PK(sbk)])
                        if has_bias:
                            s2, g2 = h // 3, h % 3
                            S.pe_drain()
                            mm(P[sbk][:, c0:CH], indb[32 * g2:32 * g2 + 32, kt // 2, :],
                               BIAS[32 * g2:32 * g2 + 32, s2, c0:CH], False, True,
                               reads=["indb"] + [("BIAS", jj) for jj in range(4)], writes=[PK(sbk)])
                        pt_i = ptc["i"] % 3
                        ptc["i"] += 1
                        pT = PT[pt_i]
                        act(pT[:, c0:CH], P[sbk][:, c0:CH], AF.Exp, reads=[PK(sbk)], writes=[("pT", pt_i)], scale=0.125)
                        if branch == 1:
                            tt("dve", pT[:, c0:CH], pT[:, c0:CH], maskT(kt, c0, CH), ALU.mult,
                               reads=[("pT", pt_i), ("U", kt)], writes=[("pT", pt_i)])
                        if pend[0] is not None:
                            pend[0]()
                        pend[0] = (lambda kt=kt, c0=c0, pT=pT, pt_i=pt_i: mm(
                            P[ov][:, c0:CH], lv(kt), pT[:, c0:CH], kt == 0, kt == nkt - 1,
                            reads=[(vn, kt, e_), vn + "1", ("pT", pt_i)], writes=[PK(ov)]))
                    pend[0]()
                    if e_ == 0:
                        S.op("dve", lambda e, ov=ov: e.reciprocal(out=rden[0:64, :], in_=P[ov][64:128, :]),
                             reads=[PK(ov)], writes=[("rden", 0)])
                        tt("dve", Ot[0:64, p_, :], P[ov][0:64, :], rden[0:64, :], ALU.mult,
                           reads=[PK(ov), ("rden", 0)], writes=[(on, p_, 0)])
                    else:
                        S.op("dve", lambda e, ov=ov: e.reciprocal(out=rden[64:128, :], in_=P[ov][0:64, :]),
                             reads=[PK(ov)], writes=[("rden", 1)])
                        tt("dve", Ot[64:128, p_, :], P[ov][64:128, :], rden[64:128, :], ALU.mult,
                           reads=[PK(ov), ("rden", 1)], writes=[(on, p_, 1)])


                for j in range(4):
                    qt = 4 * c + j
                    ns = (qt + 1) * 128
                    for h in range(8):
                        ts("pool", Dw[:, h, :], ident_b, wis[:, j, h:h + 1], IDX_SCALE, ALU.mult, ALU.mult,
                           reads=["cstb", "wis"], writes=[("Dw", h)])
                    npc = (ns + 511) // 512
                    for pc in range(npc):
                        k0 = pc * 512
                        wd = min(512, ns - k0)
                        kik = [("KI", s_i * NCH_SEQ + pc)]
                        dpend = [None]
                        for h in range(8):
                            p_, e_ = h // 2, h % 2
                            b = next_acc()
                            mm(P[b][:, 0:wd], QI[64 * e_:64 * e_ + 64, p_, j * 128:(j + 1) * 128],
                               KI[64 * e_:64 * e_ + 64, k0:k0 + wd], True, True,
                               reads=[("QI", p_)] + kik, writes=[PK(b)])
                            act(RL[h % 2][:, 0:wd], P[b][:, 0:wd], AF.Relu, reads=[PK(b)], writes=[("RL", h % 2)])
                            if dpend[0] is not None:
                                dpend[0]()
                            dpend[0] = (lambda h=h, wd=wd: mm(P[2][:, 0:wd], Dw[:, h, :], RL[h % 2][:, 0:wd], h == 0, h == 7,
                                                               reads=[("Dw", h), ("RL", h % 2)], writes=[PK(2)]))
                        dpend[0]()
                        dpend[0] = None
                        if pc == npc - 1:
                            dcol = ns - 128 - k0
                            if dcol > 0:
                                act(Ibuf[:, k0:k0 + dcol], P[2][:, 0:dcol], AF.Copy, reads=[PK(2)], writes=[("I", pc)])
                            tt("dve", Ibuf[:, ns - 128:ns], P[2][:, dcol:dcol + 128], tri_tok, ALU.add,
                               reads=[PK(2), "cstf"], writes=[("I", 4)])
                        else:
                            act(Ibuf[:, k0:k0 + 512], P[2][:, :], AF.Copy, reads=[PK(2)], writes=[("I", pc)])
                    ik = [("I", i) for i in range(5)]
                    if qt >= 2:
                        S.op("dve", lambda e, ns=ns: e.tensor_reduce(out=st[:, 4:5], in_=Ibuf[:, 0:ns], axis=AX.X, op=ALU.max),
                             reads=ik, writes=[("st", 4)])
                        S.op("dve", lambda e, ns=ns: e.tensor_reduce(out=st[:, 5:6], in_=Ibuf[:, 0:ns - 128], axis=AX.X, op=ALU.min),
                             reads=ik, writes=[("st", 5)])
                        tt("dve", st[:, 6:7], st[:, 4:5], st[:, 5:6], ALU.subtract, reads=[("st", 4), ("st", 5)], writes=[("st", 6)])
                        ts("dve", stp[:], pow2[:], st[:, 6:7], None, ALU.mult, None, reads=["pow2", ("st", 6)], writes=["stp"])
                        stt(st[:, 7:8], st[:, 6:7], 0.5, st[:, 5:6], ALU.mult, ALU.add,
                            reads=[("st", 6), ("st", 5)], writes=[("st", 7)])
                        for it in range(NIT):
                            ts("dve", maskb[:, 0:ns], Ibuf[:, 0:ns], st[:, 7:8], None, ALU.is_ge, ALU.add,
                               reads=ik + [("st", 7)], writes=["maskb", ("st", 8)], accum_out=st[:, 8:9])
                            ts("dve", st[:, 9:10], st[:, 8:9], 255.5, 0.5, ALU.is_ge, ALU.subtract,
                               reads=[("st", 8)], writes=[("st", 9)])
                            stt(st[:, 7:8], st[:, 9:10], stp[:, it:it + 1], st[:, 7:8], ALU.mult, ALU.add,
                                reads=[("st", 9), "stp", ("st", 7)], writes=[("st", 7)])
                        ts("dve", maskb[:, 0:ns], Ibuf[:, 0:ns], st[:, 7:8], None, ALU.is_ge, None,
                           reads=ik + [("st", 7)], writes=["maskb"])
                    else:
                        ts("dve", maskb[:, 0:ns], Ibuf[:, 0:ns], -1.0e29, None, ALU.is_ge, None,
                           reads=ik, writes=["maskb"])
                    attn_head(0, 2 * j)
                    attn_head(0, 2 * j + 1)
                    for k8 in range(0, qt + 1, 8):
                        n8 = min(8, qt + 1 - k8)
                        for t8 in range(n8):
                            kt = k8 + t8
                            tr(PB[:, t8 * 128:(t8 + 1) * 128], maskb[:, kt * 128:(kt + 1) * 128], ident_b,
                               reads=["maskb", "cstb"], writes=[PBK])
                        for t8 in range(n8):
                            kt = k8 + t8
                            act(maskT(kt, j * 128, (j + 1) * 128), PB[:, t8 * 128:(t8 + 1) * 128], AF.Copy,
                                reads=[PBK], writes=[("U", kt)])

                for h in range(8):
                    attn_head(1, h)

                if lvl < 5:
                    continue
                wga = [load_w(8), None]
                oak = [("OA", p_, e_) for p_ in range(4) for e_ in range(2)]
                obk = [("OB", p_, e_) for p_ in range(4) for e_ in range(2)]
                SGA = Ibuf[:].bitcast(BF16)
                SGB = maskb[:]
                ikeys = [("I", i) for i in range(5)]
                for half in range(2):
                    wga_b, wga_k = wga[0] if half == 0 else load_w(9)
                    for mm_ in range(4):
                        m = half * 4 + mm_
                        b = proj_fm(wga_b, wga_k, mm_)
                        act(SGA[:, m * 512:(m + 1) * 512], P[b][:, :], AF.Sigmoid, reads=[PK(b)], writes=ikeys)
                wpa_b, wpa_k = load_w(12)
                for m in range(8):
                    b = next_acc()
                    for p_ in range(4):
                        mm(P[b][:, :], wpa_b[:, p_ * 1024 + m * 128: p_ * 1024 + (m + 1) * 128], OA[:, p_, :],
                           p_ == 0, p_ == 3, reads=[wpa_k] + oak, writes=[PK(b)])
                    tt("dve", fsc(0), P[b][:, :], SGA[:, m * 512:(m + 1) * 512], ALU.mult,
                       reads=[PK(b)] + ikeys, writes=UK(0, 2))
                    S.op("pool", lambda e, m=m: e.tensor_copy(out=SGA[:, m * 512:(m + 1) * 512], in_=fsc(0)),
                         reads=UK(0, 2) + ikeys, writes=ikeys)
                for half in range(2):
                    wgb_b, wgb_k = load_w(10 + half)
                    for mm_ in range(4):
                        b = proj_fm(wgb_b, wgb_k, mm_)
                        act(SGB[:, mm_ * 512:(mm_ + 1) * 512], P[b][:, :], AF.Sigmoid, reads=[PK(b)], writes=["maskb"])
                    wpb_b, wpb_k = load_w(13)
                    for mm_ in range(4):
                        m = half * 4 + mm_
                        b = next_acc()
                        for p_ in range(4):
                            mm(P[b][:, :], wpb_b[:, p_ * 1024 + m * 128: p_ * 1024 + (m + 1) * 128], OB[:, p_, :],
                               p_ == 0, p_ == 3, reads=[wpb_k] + obk, writes=[PK(b)])
                        tt("dve", fsc(1), P[b][:, :], SGB[:, mm_ * 512:(mm_ + 1) * 512], ALU.mult,
                           reads=[PK(b), "maskb"], writes=UK(2, 4))
                        tt("pool", mixT[:, m, :], fsc(1), SGA[:, m * 512:(m + 1) * 512], ALU.add,
                           reads=UK(2, 4) + ikeys, writes=[("QA", m) if m < 4 else ("QB", m - 4)])
                mixk = [("QA", m) for m in range(4)] + [("QB", m) for m in range(4)]

                if lvl < 6:
                    continue
                wo = [load_w(14), load_w(15)]
                for j in range(4):
                    xt = XT[j % 2]
                    xk = ("xt", j % 2)
                    r0 = tok0 + j * 128
                    tile_i = r0 // 128
                    S.dma("sp", lambda e, xt=xt, r0=r0: e.dma_start(out=xt[:], in_=x_d[r0:r0 + 128, :]), xk, writes=[xk])
                    for nh in range(2):
                        wob, wok = wo[nh]
                        ob_ = 5 + nh
                        for m in range(8):
                            mm(P[ob_][:, :], mixT[:, m, j * 128:(j + 1) * 128], wob[:, m * 512:(m + 1) * 512],
                               m == 0, m == 7, reads=[wok] + mixk, writes=[PK(ob_)])
                        tt("dve", xt[:, nh * 512:(nh + 1) * 512], P[ob_][:, :], xt[:, nh * 512:(nh + 1) * 512], ALU.add,
                           reads=[PK(ob_), xk], writes=[xk])
                    S.dma("sp", lambda e, xt=xt, r0=r0: e.dma_start(out=x1s_d[r0:r0 + 128, :], in_=xt[:]), ("x1st", j % 2),
                          reads=[xk], writes=[("x1s", tile_i)])
                    if not do_moe:
                        continue
                    rms_stats(xt[:], xk, 0)
                    stt(xnf, xt[:], st[:, 0:1], gffn[:], ALU.mult, ALU.mult, reads=[xk, ("st", 0), "gffn"], writes=UK(8, 12))
                    xq = xnb[j % 2]
                    xqk = ("xnb", 0)
                    act(xq[:], xnf, AF.Copy, reads=UK(8, 12), writes=[xqk])
                    for hf in range(2):
                        for k4 in range(4):
                            kc = hf * 4 + k4
                            tr(P[3 + hf][:, k4 * 128:(k4 + 1) * 128], xnf[:, kc * 128:(kc + 1) * 128], ident_f,
                               reads=UK(8, 12) + ["cstf"], writes=[PK(3 + hf)])
                        act(xnT[:, hf * 512:(hf + 1) * 512], P[3 + hf][:, :], AF.Copy, reads=[PK(3 + hf)],
                            writes=UK(12 + 2 * hf, 14 + 2 * hf))
                    for kc in range(8):
                        mm(P[2][:, 0:36], xnT[:, kc * 128:(kc + 1) * 128], wr[:, kc, :], kc == 0, kc == 7,
                           reads=UK(12, 16) + ["wr"], writes=[PK(2)])
                    R_ = lambda a, b_: rt[:, a:b_]
                    rk = lambda n: ("rt", n)
                    tt("dve", R_(0, 36), P[2][:, 0:36], brs[:], ALU.add, reads=[PK(2), "brs"], writes=[rk("lg")])
                    S.op("dve", lambda e: e.tensor_reduce(out=rt[:, 36:37], in_=rt[:, 0:4], axis=AX.X, op=ALU.max),
                         reads=[rk("lg")], writes=[rk("gmax")])
                    ts("dve", R_(40, 44), R_(0, 4), R_(36, 37), None, ALU.is_ge, None, reads=[rk("lg"), rk("gmax")], writes=[rk("goh")])
                    ts("dve", R_(37, 38), R_(36, 37), -1.0, None, ALU.mult, None, reads=[rk("gmax")], writes=[rk("ngmax")])
                    act(R_(44, 48), R_(0, 4), AF.Exp, reads=[rk("lg"), rk("ngmax")], writes=[rk("gexp"), rk("gsum")],
                        bias=rt[:, 37:38], accum_out=rt[:, 38:39])
                    S.op("dve", lambda e: e.reciprocal(out=rt[:, 39:40], in_=rt[:, 38:39]), reads=[rk("gsum")], writes=[rk("gw")])
                    ts("dve", R_(48, 52), R_(40, 44), 1.0, 1.0e30, ALU.subtract, ALU.mult, reads=[rk("goh")], writes=[rk("eb")])
                    tt("dve", rt[:, 64:96].rearrange("p (a b) -> p a b", a=4), rt[:, 4:36].rearrange("p (a b) -> p a b", a=4),
                       rt[:, 48:52].rearrange("p (a b) -> p a b", b=1).to_broadcast([128, 4, 8]), ALU.add,
                       reads=[rk("lg"), rk("eb")], writes=[rk("elm")])
                    S.op("dve", lambda e: e.max(out=rt[:, 96:104], in_=rt[:, 64:96]), reads=[rk("elm")], writes=[rk("top")])
                    ts("dve", R_(128, 160), R_(64, 96), R_(96, 97), None, ALU.is_equal, None, reads=[rk("elm"), rk("top")], writes=[rk("A1")])
                    ts("dve", R_(160, 192), R_(64, 96), R_(97, 98), None, ALU.is_equal, None, reads=[rk("elm"), rk("top")], writes=[rk("A2")])
                    tt("dve", R_(104, 105), R_(97, 98), R_(96, 97), ALU.subtract, reads=[rk("top")], writes=[rk("d21")])
                    act(R_(105, 106), R_(104, 105), AF.Exp, reads=[rk("d21")], writes=[rk("e21")])
                    ts("dve", R_(106, 107), R_(105, 106), 1.0, None, ALU.add, None, reads=[rk("e21")], writes=[rk("den")])
                    S.op("dve", lambda e: e.reciprocal(out=rt[:, 107:108], in_=rt[:, 106:107]), reads=[rk("den")], writes=[rk("rden")])
                    tt("dve", GATES[:, tile_i, 0:1], R_(39, 40), R_(107, 108), ALU.mult, reads=[rk("gw"), rk("rden")], writes=[("G", tile_i, 0)])
                    tt("dve", GATES[:, tile_i, 1:2], GATES[:, tile_i, 0:1], R_(105, 106), ALU.mult,
                       reads=[("G", tile_i, 0), rk("e21")], writes=[("G", tile_i, 1)])
                    tt("dve", a12[:], R_(128, 160), R_(160, 192), ALU.add, reads=[rk("A1"), rk("A2")], writes=["a12"])
                    mm(P[2][:, 64:96], su_b, a12[:], True, True, reads=["a12", "cstb"], writes=[PK(2)])
                    mm(P[2][:, 96:128], ones_b, a12[:], True, True, reads=["a12", "cstb"], writes=[PK(2)])
                    tt("dve", R_(192, 224), P[2][:, 64:96], basec[:], ALU.add, reads=[PK(2), "basec"], writes=[rk("pos")])
                    ts("dve", R_(224, 256), R_(192, 224), float(CAP), None, ALU.is_lt, None, reads=[rk("pos")], writes=[rk("ok")])
                    tt("dve", R_(192, 224), R_(192, 224), eoff[:], ALU.add, reads=[rk("pos"), "eoff"], writes=[rk("pos")])
                    stt(R_(192, 224), R_(192, 224), -BIGIDX, R_(224, 256), ALU.add, ALU.mult, reads=[rk("pos"), rk("ok")], writes=[rk("pos")])
                    ts("dve", R_(192, 224), R_(192, 224), BIGIDX, None, ALU.add, None, reads=[rk("pos")], writes=[rk("pos")])
                    stt(R_(64, 96), R_(128, 160), 1.0, R_(192, 224), ALU.mult, ALU.mult, reads=[rk("A1"), rk("pos"), rk("elm")],
                        writes=[rk("elm"), rk("d1")], accum_out=rt[:, 108:109])
                    stt(R_(64, 96), R_(160, 192), 1.0, R_(192, 224), ALU.mult, ALU.mult, reads=[rk("A2"), rk("pos"), rk("elm")],
                        writes=[rk("elm"), rk("d2")], accum_out=rt[:, 109:110])
                    S.op("dve", lambda e, tile_i=tile_i: e.tensor_copy(out=DEST[:, tile_i, :], in_=rt[:, 108:110]),
                         reads=[rk("d1"), rk("d2")], writes=[("DEST", tile_i)])
                    tt("dve", basec[:], P[2][:, 96:128], basec[:], ALU.add, reads=[PK(2), "basec"], writes=["basec"])
                    for k in range(2):
                        S.dma("pool", lambda e, tile_i=tile_i, k=k, xq=xq: e.indirect_dma_start(
                            out=xs_d[:, :], out_offset=bass.IndirectOffsetOnAxis(ap=DEST[:, tile_i, k:k + 1], axis=0),
                            in_=xq[:, :], in_offset=None, bounds_check=S.regs["bc"], oob_is_err=False),
                            ("sc", j % 2, k), reads=[xqk, ("DEST", tile_i)], writes=[("xs",)])
            S.barrier()
            S.emit(nc, stack)

        if do_moe and not skip2:
            ph2 = ExitStack()
            with ph2:
                def sb2(name, shape, dt):
                    return ph2.enter_context(nc.sbuf_tensor(name, shape, dt))
                stg = [sb2("stg%d" % i, [128, 4096], F32) for i in range(2)]
                Wb = [[sb2("W%d_%d" % (k, i), [128, 4096], BF16) for k in range(3)] for i in range(2)]
                xsb = [sb2("xsb%d" % i, [128, D], BF16) for i in range(2 * (CAP // 128))]
                xT = sb2("xT", [128, 8, CAP], BF16)
                hidT = sb2("hidT", [128, 4, CAP], BF16)
                s1 = [sb2("s1_%d" % i, [128, 512], F32) for i in range(2)]
                yt = [sb2("yt%d" % i, [128, D], F32) for i in range(2)]
                NST = CAP // 128
                halves = [(0, 512), (512, CAP - 512)] if CAP > 512 else [(0, CAP)]
                sg = {"i": 0}

                def load_expert(ex):
                    wi_ = ex % 2
                    for k, wd_ in enumerate((w1_d, w3_d, w2_d)):
                        si = sg["i"] % 2
                        sg["i"] += 1
                        S.dma("sp", lambda e, wd_=wd_, ex=ex, si=si: e.dma_start(out=stg[si][:], in_=wd_[ex, :, :]),
                              ("stg", si), writes=[("stg", si)])
                        S.op("pool", lambda e, wi_=wi_, k=k, si=si: e.tensor_copy(out=Wb[wi_][k][:], in_=stg[si][:]),
                             reads=[("stg", si)], writes=[("W", wi_, k)])
                    for sti in range(NST):
                        r0 = ex * CAP + sti * 128
                        bi = (ex % 2) * NST + sti
                        S.dma("sp", lambda e, bi=bi, r0=r0: e.dma_start(out=xsb[bi][:], in_=xs_d[r0:r0 + 128, :]),
                              ("xsb", bi), writes=[("xsb", bi)])

                load_expert(0)
                for ex in range(NEXP):
                    wi_ = ex % 2
                    if ex + 1 < NEXP:
                        load_expert(ex + 1)
                    W1, W3, W2 = Wb[wi_]
                    for sti in range(NST):
                        bi = (ex % 2) * NST + sti
                        xq = xsb[bi]
                        for kc in range(8):
                            tr(PB[:, kc * 128:(kc + 1) * 128], xq[:, kc * 128:(kc + 1) * 128], ident_b,
                               reads=[("xsb", bi), "cstb"], writes=[PBK])
                        S.op("dve", lambda e, sti=sti: e.tensor_copy(out=xT[:, :, sti * 128:(sti + 1) * 128],
                                                                     in_=PB[:, :].rearrange("p (a b) -> p a b", a=8)),
                             reads=[PBK], writes=[("xT", sti)])
                    xTk = [("xT", i) for i in range(NST)]
                    for f in range(4):
                        for hi_, (n0, w_) in enumerate(halves):
                            for kc in range(8):
                                mm(P[0][:, 0:w_], W1[:, kc * 512 + f * 128: kc * 512 + (f + 1) * 128], xT[:, kc, n0:n0 + w_],
                                   kc == 0, kc == 7, reads=[("W", wi_, 0)] + xTk, writes=[PK(0)])
                            for kc in range(8):
                                mm(P[1][:, 0:w_], W3[:, kc * 512 + f * 128: kc * 512 + (f + 1) * 128], xT[:, kc, n0:n0 + w_],
                                   kc == 0, kc == 7, reads=[("W", wi_, 1)] + xTk, writes=[PK(1)])
                            act(s1[hi_][:, 0:w_], P[0][:, 0:w_], AF.Silu, reads=[PK(0)], writes=[("s1", hi_)])
                            tt("dve", hidT[:, f, n0:n0 + w_], P[1][:, 0:w_], s1[hi_][:, 0:w_], ALU.mult,
                               reads=[PK(1), ("s1", hi_)], writes=[("hid", f, hi_)])
                    hk = [("hid", f, hi_) for f in range(4) for hi_ in range(len(halves))]
                    for sti in range(NST):
                        r0 = ex * CAP + sti * 128
                        yq = yt[sti % 2]
                        for nh in range(2):
                            pb_ = 3 + nh
                            for f in range(4):
                                mm(P[pb_][:, :], hidT[:, f, sti * 128:(sti + 1) * 128], W2[:, f * 1024 + nh * 512: f * 1024 + (nh + 1) * 512],
                                   f == 0, f == 3, reads=[("W", wi_, 2)] + hk, writes=[PK(pb_)])
                            if nh == 0:
                                act(yq[:, 0:512], P[pb_][:, :], AF.Copy, reads=[PK(pb_)], writes=[("yt", sti % 2)])
                            else:
                                S.op("dve", lambda e, yq=yq, pb_=pb_: e.tensor_copy(out=yq[:, 512:1024], in_=P[pb_][:, :]),
                                     reads=[PK(pb_)], writes=[("yt", sti % 2)])
                        S.dma("sp", lambda e, yq=yq, r0=r0: e.dma_start(out=ys_d[r0:r0 + 128, :], in_=yq[:]),
                              ("yts", sti % 2), reads=[("yt", sti % 2)], writes=[("ys",)])
                S.barrier()
                S.emit(nc, stack)

        ph3 = ExitStack()
        with ph3:
            def sb3(name, shape, dt):
                return ph3.enter_context(nc.sbuf_tensor(name, shape, dt))
            gfin = sb3("gfin", [128, D], F32)
            y1 = [sb3("y1_%d" % i, [128, D], F32) for i in range(2)]
            y2 = [sb3("y2_%d" % i, [128, D], F32) for i in range(2)]
            x1t = [sb3("x1t%d" % i, [128, D], F32) for i in range(2)]
            ot = [sb3("ot%d" % i, [128, D], F32) for i in range(2)]
            junk = sb3("junk", [128, D], BF16)
            st3 = sb3("st3", [128, 4], F32)
            S.dma("sp", lambda e: e.dma_start(out=gfin[:], in_=gv_d[2, :, :]), "gfin", writes=["gfin"])
            for ti in range(n_chunks * 4 if p3_tiles is None else p3_tiles):
                r0 = ti * 128
                k2 = ti % 2
                S.dma("sp", lambda e, k2=k2, r0=r0: e.dma_start(out=x1t[k2][:], in_=x1s_d[r0:r0 + 128, :]),
                      ("x1l", k2), writes=[("x1t", k2)])
                acc = x1t[k2]
                if do_moe:
                    for k, yy in enumerate((y1, y2)):
                        S.op("pool", lambda e, yy=yy, k2=k2: e.memset(yy[k2][:], 0.0), writes=[("y", k, k2)])
                        S.dma("pool", lambda e, yy=yy, k2=k2, ti=ti, k=k: e.indirect_dma_start(
                            out=yy[k2][:, :], out_offset=None, in_=ys_d[:, :],
                            in_offset=bass.IndirectOffsetOnAxis(ap=DEST[:, ti, k:k + 1], axis=0),
                            bounds_check=S.regs["bc"], oob_is_err=False),
                            ("ga", k, k2), reads=[("ys",)], writes=[("y", k, k2)])
                        S.op("dve", lambda e, yy=yy, k2=k2, ti=ti, k=k, acc=acc: e.scalar_tensor_tensor(
                            out=acc[:], in0=yy[k2][:], scalar=GATES[:, ti, k:k + 1], in1=acc[:], op0=ALU.mult, op1=ALU.add),
                            reads=[("y", k, k2), ("x1t", k2)], writes=[("x1t", k2)])
                S.op("dve", lambda e, acc=acc: e.scalar_tensor_tensor(
                    out=junk[:], in0=acc[:], scalar=1.0, in1=acc[:], op0=ALU.mult, op1=ALU.mult, accum_out=st3[:, 0:1]),
                    reads=[("x1t", k2)], writes=["junk", ("st3", 0)])
                S.op("act", lambda e: e.activation(out=st3[:, 1:2], in_=st3[:, 0:1], func=AF.Ln, scale=1.0 / D, bias=epsb[:, 0:1]),
                     reads=[("st3", 0), "epsb"], writes=[("st3", 1)])
                S.op("act", lambda e: e.activation(out=st3[:, 0:1], in_=st3[:, 1:2], func=AF.Exp, scale=-0.5),
                     reads=[("st3", 1)], writes=[("st3", 0)])
                S.op("dve", lambda e, acc=acc, k2=k2: e.scalar_tensor_tensor(
                    out=ot[k2][:], in0=acc[:], scalar=st3[:, 0:1], in1=gfin[:], op0=ALU.mult, op1=ALU.mult),
                    reads=[("x1t", k2), ("st3", 0), "gfin"], writes=[("ot", k2)])
                S.dma("sp", lambda e, k2=k2, r0=r0: e.dma_start(out=out_d[r0:r0 + 128, :], in_=ot[k2][:]),
                      ("ots", k2), reads=[("ot", k2)], writes=[("out", ti)])
            S.barrier()
            S.emit(nc, stack)
    return nc


def _host_consts():
    ident = np.eye(128, dtype=np.float32)
    perm = np.zeros((128, 128), np.float32)
    for m in range(128):
        if (m % 64) < 32:
            perm[m + 32, m] = -1.0
        else:
            perm[m - 32, m] = 1.0
    k = np.arange(128)[:, None]
    q = np.arange(128)[None, :]
    tri_kq = np.where(k <= q, 0.0, NEGB).astype(np.float32)
    su = (k < q).astype(np.float32)
    ones = np.ones((128, 128), np.float32)
    tri_tok = np.where(q <= k, 0.0, -1.0e30).astype(np.float32)
    cst = np.zeros((128, 8, 128), np.float32)
    for i, a in enumerate((ident, perm, tri_kq, su, ones, tri_tok)):
        cst[:, i, :] = a
    ind = np.zeros((128, 8, 128), np.float32)
    for g in range(4):
        for n in range(8):
            ind[32 * g + n, n, :] = 1.0
    inv = np.power(10000.0, -np.arange(0, 64, 2, dtype=np.float32) / 64).astype(np.float32)
    ang = np.arange(SEQ, dtype=np.float32)[:, None] * inv[None, :]
    cosr = np.tile(np.cos(ang).astype(np.float32).T, (4, 1))
    sinr = np.tile(np.sin(ang).astype(np.float32).T, (4, 1))
    eoff = np.tile((np.arange(32, dtype=np.float32) * CAP)[None, :], (128, 1))
    pow2 = np.tile((0.5 ** np.arange(1, NIT + 1, dtype=np.float32))[None, :], (128, 1)).astype(np.float32)
    return dict(cst=cst, ind=ind, cosr=np.ascontiguousarray(cosr), sinr=np.ascontiguousarray(sinr),
                eoff=eoff, pow2=pow2)


def _kc_layout(w):
    K, N = w.shape
    return np.ascontiguousarray(w.reshape(K // 128, 128, N).transpose(1, 0, 2).reshape(128, (K // 128) * N))


def _host_weights(inp):
    w_in = inp["w_in"][0]
    sp = np.cumsum([0, 512, 512, 512, 512, 512, 512, 512, 64, 8, 1024, 1024])
    qa, ka, va, qb, kb, vb, qi, ki, wi, ga, gb = [w_in[:, sp[i]:sp[i + 1]] for i in range(11)]
    g7 = np.zeros((1024, 512), np.float32)
    g7[:, 0:64] = ki
    g7[:, 64:128] = ki
    g7[:, 128:136] = wi
    groups = [qa, ka, va, qb, kb, vb, qi, g7, ga[:, :512], ga[:, 512:], gb[:, :512], gb[:, 512:]]
    wcat = np.zeros((16, 128, 4096), np.float32)
    for i, g in enumerate(groups):
        wcat[i] = _kc_layout(g)
    wcat[12] = _kc_layout(inp["w_proj_a"][0])
    wcat[13] = _kc_layout(inp["w_proj_b"][0])
    wo = inp["w_out"][0]
    wcat[14] = _kc_layout(wo[:, :512])
    wcat[15] = _kc_layout(wo[:, 512:])
    w1r = np.stack([_kc_layout(inp["w1"][0, e]) for e in range(NEXP)])
    w3r = np.stack([_kc_layout(inp["w3"][0, e]) for e in range(NEXP)])
    w2r = np.stack([_kc_layout(inp["w2"][0, e]) for e in range(NEXP)])
    wrc = np.concatenate([inp["w_group"][0], inp["w_expert"][0]], axis=1)
    wr = _kc_layout(wrc)
    br = np.tile(np.concatenate([inp["b_group"][0], inp["b_expert"][0]])[None, :], (128, 1)).astype(np.float32)
    gv = np.stack([np.tile(inp["g_mix"][0][None, :], (128, 1)), np.tile(inp["g_ffn"][0][None, :], (128, 1)),
                   np.tile(inp["g_final"][None, :], (128, 1))]).astype(np.float32)
    return dict(wcat=wcat, w1r=w1r, w3r=w3r, w2r=w2r, wr=wr, br=br, gv=gv)


_NC_CACHE = {}


def kernel(**inputs):
    inp = {k: np.asarray(v, dtype=np.float32) for k, v in inputs.items()}
    shared = _host_consts()
    shared.update(_host_weights(inp))
    x = inp["x"].reshape(NCORE, NT, D)
    if "nc" not in _NC_CACHE:
        _NC_CACHE["nc"] = build()
    nc = _NC_CACHE["nc"]
    in_maps = []
    for i in range(NCORE):
        m = dict(shared)
        m["x"] = np.ascontiguousarray(x[i])
        in_maps.append(m)
    res = run_bass_kernel_spmd(nc, in_maps, core_ids=list(range(NCORE)))
    out = np.concatenate([np.asarray(r["out"]) for r in res.results], axis=0)
    return out.reshape(32, SEQ, D).astype(np.float32)
```
